# Optimizing a Trainium2 kernel written in Bass

```python
import math
import jax, jax.numpy as jnp
from jax import lax
import numpy as np

D_MODEL = 1024
BATCH = 8
SEQ = 4096
DEPTH = 2

HEAD_DIM = 64
N_HEADS_A = 4
N_HEADS_B = 4
N_HEADS_C = 4
N_HEADS_D = 4
MIX_WIDTH = HEAD_DIM * (N_HEADS_A + N_HEADS_B + N_HEADS_C + N_HEADS_D)
DIFF_QK_DIM = HEAD_DIM // 2
DILATED_CONFIGS = ((128, 1), (512, 4), (2048, 16))
IDX_HEADS = 16
IDX_DIM = 64
INDEX_TOPK_MAX = 256
N_EXPERTS = 32
TOP_K = 4
D_FF = D_MODEL
SWIGLU_ALPHA = 1.702
SWIGLU_LIMIT = 7.0
N_BUCKETS = 32
MAX_DISTANCE = 128
N_BIAS_HEADS = N_HEADS_B + N_HEADS_C + N_HEADS_D
Q_BLOCK = 128
MOE_BLOCK = 512
LN_EPS = 1e-5
DEEPNORM_ALPHA = (2 * DEPTH) ** 0.25
DEEPNORM_BETA = (8 * DEPTH) ** -0.25

SEG_SIZES = (
    N_HEADS_A * HEAD_DIM, N_HEADS_A * HEAD_DIM, N_HEADS_A * HEAD_DIM,
    N_HEADS_B * HEAD_DIM, N_HEADS_B * HEAD_DIM, N_HEADS_B * HEAD_DIM,
    N_HEADS_C * HEAD_DIM, N_HEADS_C * HEAD_DIM, N_HEADS_C * HEAD_DIM,
    IDX_HEADS * IDX_DIM, IDX_DIM, IDX_HEADS,
    N_HEADS_D * 2 * DIFF_QK_DIM, N_HEADS_D * 2 * DIFF_QK_DIM, N_HEADS_D * HEAD_DIM,
)
IN_COLS = sum(SEG_SIZES)
SPLIT_POINTS = tuple(int(p) for p in np.cumsum(SEG_SIZES)[:-1])

kernel_name = "hybrid_sb_dilated_dsa_diff_moe_deepnorm"


def layer_norm(x, g=None, b=None):
    xf = x.astype(jnp.float32)
    xc = xf - jnp.mean(xf, -1, keepdims=True)
    y = xc * lax.rsqrt(jnp.mean(xc * xc, -1, keepdims=True) + LN_EPS)
    if g is not None:
        y = y * g.astype(jnp.float32) + b.astype(jnp.float32)
    return y.astype(x.dtype)


def rel_bucket(dist):
    n = jnp.maximum(dist, 0)
    max_exact = N_BUCKETS // 2
    nf = jnp.maximum(n, 1).astype(jnp.float32)
    large = max_exact + (jnp.log(nf / max_exact) / math.log(MAX_DISTANCE / max_exact)
                         * (N_BUCKETS - max_exact)).astype(jnp.int32)
    large = jnp.minimum(large, N_BUCKETS - 1)
    return jnp.where(n < max_exact, n, large)


def seq_blocks(a, axis):
    t = a.shape[axis]
    a = a.reshape(a.shape[:axis] + (t // Q_BLOCK, Q_BLOCK) + a.shape[axis + 1:])
    return jnp.moveaxis(a, axis, 0)


def from_blocks(o):
    o = jnp.moveaxis(o, 0, 2)
    return o.reshape(o.shape[:2] + (-1,) + o.shape[4:])


def stick_breaking_attention(q, k, v):
    t, d = q.shape[2], q.shape[3]
    scale = d ** -0.5
    kpos = jnp.arange(t)

    def block(args):
        qi, s0 = args
        qpos = s0 + jnp.arange(Q_BLOCK)
        valid = kpos[None, :] < qpos[:, None]
        z = jnp.einsum('bhqd,bhsd->bhqs', qi, k).astype(jnp.float32) * scale
        log_fail = jnp.where(valid, jax.nn.log_sigmoid(-z), 0.0)
        after = lax.cumsum(log_fail, axis=3, reverse=True) - log_fail
        w = jnp.where(valid, jnp.exp(jax.nn.log_sigmoid(z) + after), 0.0)
        return jnp.einsum('bhqs,bhsd->bhqd', w.astype(v.dtype), v)

    starts = jnp.arange(t // Q_BLOCK) * Q_BLOCK
    return from_blocks(lax.map(block, (seq_blocks(q, 2), starts)))


def dilated_window_attention(q, k, v, bias_tab):
    t, d = q.shape[2], q.shape[3]
    scale = d ** -0.5

    def block(args):
        qi, s0 = args
        qpos = s0 + jnp.arange(Q_BLOCK)
        lses, outs = [], []
        for win, dil in DILATED_CONFIGS:
            dist = jnp.arange(win // dil + 1) * dil
            kidx = qpos[:, None] - dist[None, :]
            valid = kidx >= 0
            kidx = jnp.maximum(kidx, 0)
            kg = jnp.take(k, kidx, axis=2)
            vg = jnp.take(v, kidx, axis=2)
            bias = bias_tab[rel_bucket(dist)].T.astype(jnp.float32)
            z = jnp.einsum('bhqd,bhqmd->bhqm', qi, kg).astype(jnp.float32) * scale + bias[None, :, None, :]
            z = jnp.where(valid[None, None], z, -jnp.inf)
            zmax = jnp.max(z, -1, keepdims=True)
            p = jnp.exp(z - zmax)
            den = jnp.sum(p, -1, keepdims=True)
            outs.append(jnp.einsum('bhqm,bhqmd->bhqd', (p / den).astype(v.dtype), vg))
            lses.append(zmax + jnp.log(den))
        mix = jax.nn.softmax(jnp.concatenate(lses, -1), axis=-1)
        return jnp.einsum('bhqc,cbhqd->bhqd', mix.astype(v.dtype), jnp.stack(outs))

    starts = jnp.arange(t // Q_BLOCK) * Q_BLOCK
    return from_blocks(lax.map(block, (seq_blocks(q, 2), starts)))


def dsa_attention(q, k, v, q_idx, k_idx, w_idx, bias_tab):
    t, d = q.shape[2], q.shape[3]
    topk = min(INDEX_TOPK_MAX, t // 4)
    scale = d ** -0.5
    kpos = jnp.arange(t)

    def block(args):
        qi, qxi, wxi, s0 = args
        qpos = s0 + jnp.arange(Q_BLOCK)
        dots = jnp.einsum('bqhd,bsd->bqhs', qxi, k_idx).astype(jnp.float32) * IDX_DIM ** -0.5
        score = jnp.einsum('bqh,bqhs->bqs', wxi.astype(jnp.float32) * IDX_HEADS ** -0.5, jax.nn.relu(dots))
        score = jnp.where((kpos[None, :] <= qpos[:, None])[None], score, -jnp.inf)
        _, sel = lax.top_k(score, topk)
        kg = jax.vmap(lambda kk, ss: kk[:, ss])(k, sel)
        vg = jax.vmap(lambda vv, ss: vv[:, ss])(v, sel)
        dist = qpos[None, :, None] - sel
        bias = jnp.moveaxis(bias_tab[rel_bucket(dist)], -1, 1).astype(jnp.float32)
        z = jnp.einsum('bhqd,bhqkd->bhqk', qi, kg).astype(jnp.float32) * scale + bias
        z = jnp.where((dist >= 0)[:, None], z, -jnp.inf)
        p = jax.nn.softmax(z, axis=-1)
        return jnp.einsum('bhqk,bhqkd->bhqd', p.astype(v.dtype), vg)

    starts = jnp.arange(t // Q_BLOCK) * Q_BLOCK
    xs = (seq_blocks(q, 2), seq_blocks(q_idx, 1), seq_blocks(w_idx, 1), starts)
    return from_blocks(lax.map(block, xs))


def differential_attention(q1, q2, k1, k2, v, lam, bias_tab):
    t = q1.shape[2]
    scale = q1.shape[-1] ** -0.5
    kpos = jnp.arange(t)

    def block(args):
        q1i, q2i, s0 = args
        qpos = s0 + jnp.arange(Q_BLOCK)
        dist = qpos[:, None] - kpos[None, :]
        causal = dist >= 0
        bias = jnp.moveaxis(bias_tab[rel_bucket(dist)], -1, 0).astype(jnp.float32)

        def probs(qi, kk):
            z = jnp.einsum('bhqd,bhsd->bhqs', qi, kk).astype(jnp.float32) * scale + bias[None]
            return jax.nn.softmax(jnp.where(causal, z, -jnp.inf), axis=-1)

        a = probs(q1i, k1) - lam * probs(q2i, k2)
        return jnp.einsum('bhqs,bhsd->bhqd', a.astype(v.dtype), v)

    starts = jnp.arange(t // Q_BLOCK) * Q_BLOCK
    return from_blocks(lax.map(block, (seq_blocks(q1, 2), seq_blocks(q2, 2), starts)))


def hybrid_mixer(h, w_in, w_out, diff_lam, diff_g, rel_bias, layer_idx):
    b, t, _ = h.shape
    (qa, ka, va, qb, kb, vb, qc, kc, vc, qx, kx, wx, qd, kd, vd) = jnp.split(h @ w_in, SPLIT_POINTS, axis=-1)

    def heads(a, n):
        return a.reshape(b, t, n, -1).transpose(0, 2, 1, 3)

    def merge(o):
        return o.transpose(0, 2, 1, 3).reshape(b, t, -1)

    bias_b = rel_bias[:, :N_HEADS_B]
    bias_c = rel_bias[:, N_HEADS_B:N_HEADS_B + N_HEADS_C]
    bias_d = rel_bias[:, N_HEADS_B + N_HEADS_C:]

    o_a = stick_breaking_attention(heads(qa, N_HEADS_A), heads(ka, N_HEADS_A), heads(va, N_HEADS_A))
    o_b = dilated_window_attention(heads(qb, N_HEADS_B), heads(kb, N_HEADS_B), heads(vb, N_HEADS_B), bias_b)
    o_c = dsa_attention(heads(qc, N_HEADS_C), heads(kc, N_HEADS_C), heads(vc, N_HEADS_C),
                        qx.reshape(b, t, IDX_HEADS, IDX_DIM), kx, wx, bias_c)

    qd = qd.reshape(b, t, N_HEADS_D, 2, DIFF_QK_DIM)
    kd = kd.reshape(b, t, N_HEADS_D, 2, DIFF_QK_DIM)
    q1, q2 = qd[..., 0, :].transpose(0, 2, 1, 3), qd[..., 1, :].transpose(0, 2, 1, 3)
    k1, k2 = kd[..., 0, :].transpose(0, 2, 1, 3), kd[..., 1, :].transpose(0, 2, 1, 3)
    lamp = diff_lam.astype(jnp.float32)
    lambda_init = 0.8 - 0.6 * math.exp(-0.3 * layer_idx)
    lam = jnp.exp(jnp.sum(lamp[0] * lamp[1])) - jnp.exp(jnp.sum(lamp[2] * lamp[3])) + lambda_init
    o_d = differential_attention(q1, q2, k1, k2, heads(vd, N_HEADS_D), lam, bias_d)
    of = o_d.astype(jnp.float32)
    of = of * lax.rsqrt(jnp.mean(of * of, -1, keepdims=True) + LN_EPS)
    o_d = (of * diff_g.astype(jnp.float32) * (1.0 - lambda_init)).astype(h.dtype)

    merged = jnp.concatenate([merge(o_a), merge(o_b), merge(o_c), merge(o_d)], axis=-1)
    return merged @ w_out


def clamped_swiglu(hh):
    glu, lin = hh[..., ::2], hh[..., 1::2]
    glu = jnp.minimum(glu, SWIGLU_LIMIT)
    lin = jnp.clip(lin, -SWIGLU_LIMIT, SWIGLU_LIMIT)
    return glu * jax.nn.sigmoid(SWIGLU_ALPHA * glu) * (lin + 1.0)


def moe_ffn(h, w_router, b_router, w1, b1, w2, b2):
    b, t, d = h.shape
    hf = h.reshape(-1, d)
    n_tok = hf.shape[0]
    logits = (hf @ w_router + b_router).astype(jnp.float32)
    top_val, top_idx = lax.top_k(logits, TOP_K)
    gate = jax.nn.softmax(top_val, axis=-1)
    e_flat = top_idx.reshape(-1)
    tok_flat = jnp.repeat(jnp.arange(n_tok), TOP_K)
    g_flat = gate.reshape(-1)
    m = e_flat.shape[0]
    order = jnp.argsort(e_flat)
    e_s, tok_s, g_s = e_flat[order], tok_flat[order], g_flat[order]
    counts = jnp.bincount(e_flat, length=N_EXPERTS)
    padded = (counts + MOE_BLOCK - 1) // MOE_BLOCK * MOE_BLOCK
    start = jnp.cumsum(counts) - counts
    pend = jnp.cumsum(padded)
    pstart = pend - padded
    dest = pstart[e_s] + (jnp.arange(m) - start[e_s])
    cap = ((m + MOE_BLOCK - 1) // MOE_BLOCK + N_EXPERTS) * MOE_BLOCK
    n_blocks = cap // MOE_BLOCK
    tok_buf = jnp.zeros((cap,), jnp.int32).at[dest].set(tok_s)
    g_buf = jnp.zeros((cap,), jnp.float32).at[dest].set(g_s)
    blk_expert = jnp.minimum(jnp.searchsorted(pend, jnp.arange(n_blocks) * MOE_BLOCK, side='right'),
                             N_EXPERTS - 1)

    def expert_block(args):
        tok, g, e = args
        xb = hf[tok]
        y = clamped_swiglu(xb @ w1[e] + b1[e]) @ w2[e] + b2[e]
        return y * g[:, None].astype(y.dtype)

    y = lax.map(expert_block, (tok_buf.reshape(n_blocks, MOE_BLOCK), g_buf.reshape(n_blocks, MOE_BLOCK), blk_expert))
    out = jnp.zeros_like(hf).at[tok_buf].add(y.reshape(cap, d))
    return out.reshape(b, t, d)


def setup_inputs(seed: int = 0) -> dict:
    key = jax.random.key(seed)
    ks = jax.random.split(key, 17)

    def nrm(k, shape, scale):
        return jax.random.normal(k, shape, jnp.float32) * scale

    return {
        "x": nrm(ks[0], (BATCH, SEQ, D_MODEL), 1.0),
        "c": nrm(ks[1], (BATCH, D_MODEL), 1.0),
        "w_ada": nrm(ks[2], (DEPTH, D_MODEL, 6 * D_MODEL), 0.1 * D_MODEL ** -0.5),
        "b_ada": nrm(ks[3], (DEPTH, 6 * D_MODEL), 0.02),
        "w_in": nrm(ks[4], (DEPTH, D_MODEL, IN_COLS), D_MODEL ** -0.5),
        "w_out": nrm(ks[5], (DEPTH, MIX_WIDTH, D_MODEL), DEEPNORM_BETA * MIX_WIDTH ** -0.5),
        "diff_lam": nrm(ks[6], (DEPTH, 4, DIFF_QK_DIM), 0.1),
        "diff_g": 1.0 + nrm(ks[7], (DEPTH, HEAD_DIM), 0.02),
        "ln_g": 1.0 + nrm(ks[8], (DEPTH, 2, D_MODEL), 0.02),
        "ln_b": nrm(ks[9], (DEPTH, 2, D_MODEL), 0.02),
        "w_router": nrm(ks[10], (DEPTH, D_MODEL, N_EXPERTS), D_MODEL ** -0.5),
        "b_router": nrm(ks[11], (DEPTH, N_EXPERTS), 0.01),
        "w1": nrm(ks[12], (DEPTH, N_EXPERTS, D_MODEL, 2 * D_FF), D_MODEL ** -0.5),
        "b1": nrm(ks[13], (DEPTH, N_EXPERTS, 2 * D_FF), 0.01),
        "w2": nrm(ks[14], (DEPTH, N_EXPERTS, D_FF, D_MODEL), DEEPNORM_BETA * D_FF ** -0.5),
        "b2": nrm(ks[15], (DEPTH, N_EXPERTS, D_MODEL), 0.01),
        "rel_bias": nrm(ks[16], (N_BUCKETS, N_BIAS_HEADS), 0.5),
    }


def reference(x, c, w_ada, b_ada, w_in, w_out, diff_lam, diff_g, ln_g, ln_b,
              w_router, b_router, w1, b1, w2, b2, rel_bias):
    for l in range(DEPTH):
        mod = (c @ w_ada[l] + b_ada[l])[:, None, :]
        sh1, sc1, g1, sh2, sc2, g2 = jnp.split(mod, 6, axis=-1)
        h = layer_norm(x) * (1.0 + sc1) + sh1
        y = hybrid_mixer(h, w_in[l], w_out[l], diff_lam[l], diff_g[l], rel_bias, l)
        x = layer_norm(DEEPNORM_ALPHA * x + (1.0 + g1) * y, ln_g[l, 0], ln_b[l, 0])
        h = layer_norm(x) * (1.0 + sc2) + sh2
        y = moe_ffn(h, w_router[l], b_router[l], w1[l], b1[l], w2[l], b2[l])
        x = layer_norm(DEEPNORM_ALPHA * x + (1.0 + g2) * y, ln_g[l, 1], ln_b[l, 1])
    return x
```

```python
import contextlib
import numpy as np
import concourse.bass as bass
import concourse.mybir as mybir
from concourse.bass_utils import run_bass_kernel_spmd

F32 = mybir.dt.float32
BF16 = mybir.dt.bfloat16
I32 = mybir.dt.int32
U32 = mybir.dt.uint32
AF = mybir.ActivationFunctionType
ALU = mybir.AluOpType
AX = mybir.AxisListType

T = 4096
D = 1024
NB = 32
DEPTH = 2
NE = 32
CAP = 1536
ALPHA = (2 * DEPTH) ** 0.25
LN_EPS = 1e-5
FM_COLS = 3136
V_COLS = 1024
ENGS = ("pe", "act", "dve", "pool", "sp")


class Res:
    __slots__ = ("name", "lw", "rd")

    def __init__(self, name):
        self.name = name
        self.lw = None
        self.rd = []


class Op:
    __slots__ = ("eng", "fn", "deps", "is_dma", "sem", "val", "signal", "ep")


class Prog:
    def __init__(self, nc):
        self.nc = nc
        self.q = {e: [] for e in ENGS}
        self.ops = []
        self.dma_cnt = {}
        self.pending_dma = []
        self.last_compute = {}
        self.epoch = 0

    def res(self, name="r"):
        return Res(name)

    def _deps(self, reads, writes):
        deps = []
        for r in reads:
            if r.lw is not None:
                deps.append(r.lw)
        for w in writes:
            if w.lw is not None:
                deps.append(w.lw)
            deps.extend(w.rd)
        return deps

    def _commit(self, o, reads, writes):
        for r in reads:
            r.rd.append(o)
        for w in writes:
            w.lw = o
            w.rd = []

    def op(self, eng, fn, reads=(), writes=(), extra_deps=()):
        o = Op()
        o.eng, o.fn, o.is_dma, o.signal = eng, fn, False, False
        o.ep = self.epoch
        o.deps = self._deps(reads, writes) + list(extra_deps)
        self.q[eng].append(o)
        self.ops.append(o)
        self._commit(o, reads, writes)
        self.last_compute[eng] = o
        return o

    def dma(self, eng, fn, semkey, reads=(), writes=()):
        o = Op()
        o.eng, o.fn, o.is_dma, o.signal = eng, fn, True, True
        o.ep = self.epoch
        o.deps = self._deps(reads, writes)
        self.dma_cnt[semkey] = self.dma_cnt.get(semkey, 0) + 16
        o.sem, o.val = semkey, self.dma_cnt[semkey]
        self.q[eng].append(o)
        self.ops.append(o)
        self._commit(o, reads, writes)
        self.pending_dma.append(o)
        return o

    def barrier(self):
        marks = []
        dm = {}
        for o in self.pending_dma:
            dm[o.sem] = o
        dlist = list(dm.values())
        self.pending_dma = []
        for e in ("pe", "act", "dve", "pool"):
            extra = list(dlist)
            if e in self.last_compute:
                extra.append(self.last_compute[e])
            marks.append(self.op(e, lambda g: g.nop(), extra_deps=extra))
        for e in ENGS:
            self.op(e, lambda g: g.nop(), extra_deps=marks)

    def emit(self, final_waits=()):
        nc = self.nc
        for o in self.ops:
            for d in o.deps:
                if not d.is_dma:
                    d.signal = True
        for e in ENGS:
            c = {}
            for o in self.q[e]:
                if not o.is_dma and o.signal:
                    c[o.ep] = c.get(o.ep, 0) + 1
                    o.val = c[o.ep]
                    o.sem = ("eng", e, o.ep)
        semkeys = []
        seen = set()
        for o in self.ops:
            if o.signal and o.sem not in seen:
                seen.add(o.sem)
                semkeys.append(o.sem)
        run_cnt = {}
        waits = {}
        for o in self.ops:
            w = {}
            for d in o.deps:
                v = run_cnt[d.sem] if d.is_dma else d.val
                if w.get(d.sem, 0) < v:
                    w[d.sem] = v
            waits[id(o)] = w
            if o.is_dma:
                run_cnt[o.sem] = o.val
        self.n_sems = len(semkeys)
        with contextlib.ExitStack() as st:
            sems = {}
            for i, k in enumerate(semkeys):
                sems[k] = st.enter_context(nc.semaphore("s%d" % i))
            block = st.enter_context(nc.Block())
            fin = {}
            for o in final_waits:
                fin[o.sem] = max(fin.get(o.sem, 0), run_cnt[o.sem])

            def run_queue(e, g, final=False):
                waited = {}
                for o in self.q[e]:
                    for k, v in waits[id(o)].items():
                        if waited.get(k, 0) < v:
                            g.wait_ge(sems[k], v)
                            waited[k] = v
                    ins = o.fn(g)
                    if o.is_dma:
                        ins.then_inc(sems[o.sem], 16)
                    elif o.signal:
                        ins.then_inc(sems[o.sem], 1)
                if final:
                    for k, v in fin.items():
                        g.wait_ge(sems[k], v)

            @block.tensor
            def _(g):
                run_queue("pe", g)

            @block.scalar
            def _(g):
                run_queue("act", g)

            @block.vector
            def _(g):
                run_queue("dve", g)

            @block.gpsimd
            def _(g):
                run_queue("pool", g)

            @block.sync
            def _(g):
                run_queue("sp", g, final=True)


class Buf:
    __slots__ = ("t", "r")

    def __init__(self, t, r):
        self.t = t
        self.r = r


class _T:
    def __init__(self, ap):
        self._ap = ap

    def ap(self):
        return self._ap


class View:
    def __init__(self, buf, ap, final=False):
        self.r = buf.r
        self.t = _T(ap)
        self.final = final


class Ring:
    def __init__(self, bufs):
        self.bufs = bufs
        self.i = 0

    def next(self):
        b = self.bufs[self.i % len(self.bufs)]
        self.i += 1
        return b


def rev_ap(ap, n):
    return bass.AP(tensor=ap.tensor, offset=ap.offset + n - 1, ap=[list(ap.ap[0]), [-1, n]])


def _rel_bucket(n):
    n = np.maximum(n, 0)
    nf = np.maximum(n, 1).astype(np.float32)
    large = 16 + (np.log(nf / np.float32(16)) / np.float32(np.log(128 / 16)) * np.float32(16)).astype(np.int32)
    large = np.minimum(large, 31)
    return np.where(n < 16, n, large)


def _static_tables():
    nb = 2304
    d = np.arange(nb) - 127
    buck = _rel_bucket(d)
    mult = ((d >= 0) & (d <= 128)).astype(np.float32) + ((d >= 0) & (d % 4 == 0) & (d <= 512)) + ((d >= 0) & (d % 16 == 0) & (d <= 2048))
    ohb = np.zeros((32, nb), np.float32)
    ohb[buck, np.arange(nb)] = mult
    nc_ = 384
    d2 = np.arange(nc_) - 127
    ohc = np.zeros((32, nc_), np.float32)
    ohc[_rel_bucket(d2), np.arange(nc_)] = (d2 >= 0).astype(np.float32)
    return ohb, ohc


class Builder:
    SCH = 512
    def __init__(self, dbg=(), layers=(0, 1), phases=None, stub=False, shard=False, nb=1):
        self.stub = stub
        self.shard = shard
        self.nbat = nb
        self.dbg = set(dbg)
        self.layers = layers
        self.phases = phases
        nc = bass.Bass("TRN2", target_bir_lowering=False)
        self.nc = nc
        self.P = Prog(nc)
        self.sb_off = 16512
        self.nsb = 4
        self.sb_mark = None
        self.outs = []
        self.uid = 0

    def sb(self, shape, dt, name=None):
        self.uid += 1
        esz = 4 if dt in (F32, I32, U32) else 2
        n = 1
        for s in shape[1:]:
            n *= s
        nbytes = (n * esz + 31) // 32 * 32
        t = self.nc.alloc_sbuf_tensor_at("%s_%d" % (name or "t", self.uid), list(shape), dt, offset=self.sb_off)
        self.sb_off += nbytes
        assert self.sb_off <= 229344, "SBUF overflow %d" % self.sb_off
        return Buf(t, self.P.res(name))

    def mark(self):
        self.sb_mark = self.sb_off

    def release(self):
        self.P.barrier()
        self.sb_off = self.sb_mark

    def dram(self, name, shape, dt, kind="Internal"):
        if name in self.dbg:
            kind = "ExternalOutput"
        t = self.nc.dram_tensor(name, list(shape), dt, kind=kind)
        return Buf(t, self.P.res(name))

    def ld(self, dst, dst_ap, src, src_ap, key, q="sp"):
        return self.P.dma(q, lambda g, a=dst_ap, b=src_ap: g.dma_start(out=a, in_=b), key, reads=[src.r], writes=[dst.r])

    def st(self, dst, dst_ap, src, src_ap, key, q="sp"):
        return self.P.dma(q, lambda g, a=dst_ap, b=src_ap: g.dma_start(out=a, in_=b), key, reads=[src.r], writes=[dst.r])

    def build(self):
        nc, P = self.nc, self.P
        ext = lambda name, shape, dt: Buf(nc.dram_tensor(name, list(shape), dt, kind="ExternalInput"), P.res(name))
        self.x_in = ext("x", [self.nbat, T, D], F32)
        self.cbc_all = ext("cbc", [self.nbat, 128, 8, 128], F32)
        NW = FM_COLS + V_COLS + 16
        ne_ = 1 if self.stub else NE
        if self.shard:
            self.w_ada_s = ext("w_ada", [DEPTH, 128, 6 * D], F32)
            self.w_in_s = ext("w_in", [DEPTH, 128, NW], F32)
            self.w_out_s = ext("w_out", [DEPTH, 128, D], F32)
            self.w1_s = ext("w1", [DEPTH, 4, D, 2 * D], F32)
            self.w2_s = ext("w2", [DEPTH, 4, D, D], F32)
        else:
            self.w_ada = ext("w_ada", [DEPTH, D, 6 * D], F32)
            self.w_in = ext("w_in", [DEPTH, D, NW], F32)
            self.w_out = ext("w_out", [DEPTH, D, D], F32)
            self.w1 = ext("w1", [DEPTH, ne_, D, 2 * D], F32)
            self.w2 = ext("w2", [DEPTH, ne_, D, D], F32)
        self.b_ada = ext("b_ada", [DEPTH, 6 * D], F32)
        self.diff_lam = ext("diff_lam", [DEPTH, 128], F32)
        self.diff_g = ext("diff_g", [DEPTH, 64], F32)
        self.ln_g = ext("ln_g", [DEPTH, 2, D], F32)
        self.ln_b = ext("ln_b", [DEPTH, 2, D], F32)
        self.w_router = ext("w_router", [DEPTH, D, NE], F32)
        self.b_router = ext("b_router", [DEPTH, NE], F32)
        self.b1 = ext("b1", [DEPTH, NE, 128, 16], F32)
        self.b2 = ext("b2", [DEPTH, NE, D], F32)
        self.rel_bias = ext("rel_bias", [32, 12], F32)
        self.ohb = ext("ohb", [32, 2304], F32)
        self.ohc = ext("ohc", [32, 384], F32)
        self.out = Buf(nc.dram_tensor("out", [self.nbat, T, D], F32, kind="ExternalOutput"), P.res("out"))
        self.QT = self.dram("QT", [FM_COLS, T], BF16)
        self.VT = self.dram("VT", [T, V_COLS], BF16)
        self.WX = self.dram("WX", [T, 16], F32)
        self.MT = self.dram("MT", [T, D], BF16)
        self.X1 = self.dram("X1", [T, D], F32)
        self.XL = self.dram("XL", [T, D], F32)
        ng_ = 128 if self.stub else NE * CAP
        self.XG = self.dram("XG", [ng_, D], BF16)
        self.YG = self.dram("YG", [ng_, D], F32)
        self.MV = self.dram("MV", [12, 2304], F32)
        self.pb = [Buf(nc.alloc_psum_tensor("pb%d" % i, [128, 512], F32), P.res("pb%d" % i)) for i in range(8)]

        self.consts()
        if self.shard:
            self.phase_gather()
        self.phase_bias()
        for b in range(self.nbat):
            self.cbc = View(self.cbc_all, self.cbc_all.t.ap()[b])
            for l in self.layers:
                xin = View(self.x_in, self.x_in.t.ap()[b]) if l == self.layers[0] else self.XL
                xout = View(self.out, self.out.t.ap()[b], final=True) if l == self.layers[-1] else self.XL
                if "XL" in self.dbg:
                    xout = self.XL
                self.layer(l, xin, xout)
                self.P.epoch += 1
        P.emit(final_waits=self.outs)
        return nc

    def consts(self):
        nc, P = self.nc, self.P
        self.ident = self.sb([128, 128], BF16, "ident")
        self.ident32 = self.sb([128, 128], F32, "ident32")
        self.tri_le = self.sb([128, 128], BF16, "tri_le")
        self.tri_lt = self.sb([128, 128], BF16, "tri_lt")
        self.tri_lt32 = self.sb([128, 128], F32, "tri_lt32")
        self.negm = self.sb([128, 128], F32, "negm")
        self.ut = self.sb([128, 128], F32, "ut")
        self.ones_bf = self.sb([128, 128], BF16, "ones_bf")
        self.ones32 = self.sb([128, 512], F32, "ones32")
        self.iota32 = self.sb([128, 32], F32, "iota32")
        self.pow2 = self.sb([128, 24], F32, "pow2")
        self.zero_bf = self.sb([128, 2048], BF16, "zero_bf")
        self.epsb = self.sb([128, 1], F32, "epsb")
        self.oneb = self.sb([128, 1], F32, "oneb")

        def mk(buf, val, pattern, cmp, fill, base, cm, n=128):
            P.op("pool", lambda g: g.memset(buf.t[:], val), writes=[buf.r])
            P.op("pool", lambda g: g.affine_select(out=buf.t[:], in_=buf.t[:], pattern=pattern, compare_op=cmp, fill=fill, base=base, channel_multiplier=cm),
                 reads=[buf.r], writes=[buf.r])
        mk(self.ident, 1.0, [[-1, 128]], ALU.is_equal, 0.0, 0, 1)
        mk(self.ident32, 1.0, [[-1, 128]], ALU.is_equal, 0.0, 0, 1)
        mk(self.tri_le, 1.0, [[-1, 128]], ALU.is_ge, 0.0, 0, 1)
        mk(self.tri_lt, 1.0, [[-1, 128]], ALU.is_gt, 0.0, 0, 1)
        mk(self.tri_lt32, 1.0, [[-1, 128]], ALU.is_gt, 0.0, 0, 1)
        mk(self.negm, 0.0, [[-1, 128]], ALU.is_ge, -1e30, 0, 1)
        mk(self.ut, 1.0, [[1, 128]], ALU.is_gt, 0.0, 0, -1)
        P.op("pool", lambda g: g.memset(self.ones_bf.t[:], 1.0), writes=[self.ones_bf.r])
        P.op("pool", lambda g: g.memset(self.ones32.t[:], 1.0), writes=[self.ones32.r])
        P.op("pool", lambda g: g.memset(self.zero_bf.t[:], 0.0), writes=[self.zero_bf.r])
        P.op("pool", lambda g: g.memset(self.epsb.t[:], LN_EPS), writes=[self.epsb.r])
        P.op("pool", lambda g: g.memset(self.oneb.t[:], 1.0), writes=[self.oneb.r])
        P.op("pool", lambda g: g.iota(self.iota32.t[:], pattern=[[1, 32]], base=0, channel_multiplier=0, allow_small_or_imprecise_dtypes=True), writes=[self.iota32.r])
        for i in range(24):
            P.op("pool", lambda g, i=i: g.memset(self.pow2.t[:, i:i + 1], 2.0 ** -(i + 1)), writes=[self.pow2.r])
        xg = self.XG.t.ap().rearrange("(n p) d -> p n d", p=128)
        zb = self.zero_bf.t[:].rearrange("p (n d) -> p n d", n=2)
        for i in range(0, 0 if self.stub else NE * CAP // 128, 2):
            self.st(self.XG, xg[:, i:i + 2, :], self.zero_bf, zb, "xgz")

    def layer(self, l, xin, xout):
        ph = self.phases
        base = self.sb_off
        self.mod = self.sb([128, 6 * D], F32, "mod")
        self.idx4 = self.sb([128, NB, 4], I32, "idx4")
        self.g4 = self.sb([128, NB, 4], F32, "g4")
        if ph is None or "mod" in ph:
            self.phase_mod(l)
        if ph is None or "inproj" in ph:
            self.phase_inproj(l, xin)
        if ph is None or "attA" in ph:
            self.phase_attA(l)
        if ph is None or "attB" in ph:
            self.phase_attB(l)
        if ph is None or "attC" in ph:
            self.phase_attC(l)
        if ph is None or "attD" in ph:
            self.phase_attD(l)
        if ph is None or "outproj" in ph:
            self.phase_outproj(l, xin)
        if ph is None or "experts" in ph:
            self.phase_experts(l)
        if ph is None or "combine" in ph:
            self.phase_combine(l, xout)
        self.P.barrier()
        self.sb_off = base

    def phase_mod(self, l):
        nc, P = self.nc, self.P
        self.mark()
        cbc = self.sb([128, 8, 128], F32, "cbc")
        self.ld(cbc, cbc.t[:], self.cbc, self.cbc.t.ap(), "cbc")
        wring = Ring([self.sb([128, 8, 512], F32, "wada") for _ in range(2)])
        mod = self.mod
        self.ld(mod, mod.t[:], self.b_ada, self.b_ada.t.ap()[l:l + 1, :].partition_broadcast(128), "modb")
        wb, wap = self.wsrc("w_ada", l)
        wsrc = wap.rearrange("(kc p) n -> p kc n", p=128)
        for n in range(12):
            w = wring.next()
            self.ld(w, w.t[:], wb, wsrc[:, :, n * 512:(n + 1) * 512], "wada%d" % (n % 2))
            pb = self.pb[n % 2]
            for kc in range(8):
                P.op("pe", lambda g, w=w, kc=kc, pb=pb: g.matmul(pb.t[:], lhsT=cbc.t[:, kc, :], rhs=w.t[:, kc, :], start=(kc == 0), stop=(kc == 7)),
                     reads=[cbc.r, w.r], writes=[pb.r])
            P.op("dve", lambda g, n=n, pb=pb: g.tensor_tensor(out=mod.t[:, n * 512:(n + 1) * 512], in0=pb.t[:], in1=mod.t[:, n * 512:(n + 1) * 512], op=ALU.add),
                 reads=[pb.r, mod.r], writes=[mod.r])
        for k in (1, 2, 4, 5):
            P.op("dve", lambda g, k=k: g.tensor_scalar(out=mod.t[:, k * D:(k + 1) * D], in0=mod.t[:, k * D:(k + 1) * D], scalar1=1.0, scalar2=None, op0=ALU.add),
                 reads=[mod.r], writes=[mod.r])
        if "mod" in self.dbg:
            o = self.dram("mod", [128, 6 * D], F32)
            self.outs.append(self.st(o, o.t.ap(), mod, mod.t[:], "dbg"))
        self.release()

    @property
    def wq(self):
        return "sp" if self.shard else "pool"

    def wsrc(self, name, l, e=None):
        if not self.shard:
            b = getattr(self, name)
            ap = b.t.ap()[l] if e is None else b.t.ap()[l, e]
            return b, ap
        b = self.full[name][l]
        if e is None:
            return b, b.t.ap()
        rows = D
        return b, b.t.ap()[e * rows:(e + 1) * rows, :]

    def phase_gather(self):
        nc, P = self.nc, self.P
        self.mark()
        NW = FM_COLS + V_COLS + 16
        specs = [("w_ada", self.w_ada_s, 128, 6 * D, F32), ("w_in", self.w_in_s, 128, NW, BF16), ("w_out", self.w_out_s, 128, D, BF16),
                 ("w1", self.w1_s, 4 * D, 2 * D, BF16), ("w2", self.w2_s, 4 * D, D, BF16)]
        self.full = {}
        stg = Ring([self.sb([128, 8192], BF16, "cst") for _ in range(2)])
        stg32 = Ring([self.sb([128, 6 * D], F32, "cst32") for _ in range(1)])
        k = 0
        for name, src, rows, cols, dt in specs:
            self.full[name] = []
            for l in range(DEPTH):
                shard = self.dram("%s_sh%d" % (name, l), [rows, cols], dt)
                full = self.dram("%s_full%d" % (name, l), [8 * rows, cols], dt)
                n_el = rows * cols
                if dt == F32:
                    t = stg32.next()
                    self.ld(t, t.t[:], src, src.t.ap()[l], "g32")
                    self.st(shard, shard.t.ap(), t, t.t[:], "gst")
                else:
                    per = n_el // 128
                    sflat = src.t.ap()[l].rearrange("a b -> (a b)") if len(src.t.ap()[l].shape) == 2 else src.t.ap()[l].rearrange("e a b -> (e a b)")
                    sflat = sflat.rearrange("(p j) -> p j", p=128)
                    dflat = shard.t.ap().rearrange("a b -> (a b)").rearrange("(p j) -> p j", p=128)
                    for j0 in range(0, per, 8192):
                        w = min(8192, per - j0)
                        t = stg.next()
                        self.ld(t, t.t[:, 0:w], src, sflat[:, j0:j0 + w], "gc%d" % (k % 2), q="pool")
                        self.st(shard, dflat[:, j0:j0 + w], t, t.t[:, 0:w], "gst")
                        k += 1
                P.dma("pool", lambda g, a=shard.t.ap(), b=full.t.ap(): g.collective_compute("AllGather", ALU.bypass, replica_groups=[list(range(8))], ins=[a], outs=[b]),
                      "cc", reads=[shard.r], writes=[full.r])
                self.full[name].append(full)
        self.release()

    def bcreg(self, g, val):
        if not hasattr(self, "_bcregs"):
            self._bcregs = {}
        if val not in self._bcregs:
            reg = g.alloc_register("bc%d" % val)
            g.reg_mov(reg, val)
            self._bcregs[val] = reg
        return self._bcregs[val]

    def pbf(self, i):
        return self.pb[i].t[:].bitcast(BF16)

    def evac(self, dst, dst_ap, src, src_ap, eng=None):
        self._ev = getattr(self, "_ev", 0) + 1
        if eng is None:
            eng = "act" if self._ev % 2 else "dve"
        if eng == "act":
            return self.P.op("act", lambda g, a=dst_ap, b=src_ap: g.copy(out=a, in_=b), reads=[src.r], writes=[dst.r])
        return self.P.op(eng, lambda g, a=dst_ap, b=src_ap: g.tensor_copy(out=a, in_=b), reads=[src.r], writes=[dst.r])

    def ln_rows(self, x, x_ap, st, extra_reads=()):
        P = self.P
        P.op("dve", lambda g: g.bn_stats(out=st.t[:, 16:22], in_=x_ap[:, 0:512]), reads=[x.r] + list(extra_reads), writes=[st.r])
        P.op("dve", lambda g: g.bn_stats(out=st.t[:, 22:28], in_=x_ap[:, 512:1024]), reads=[x.r, st.r], writes=[st.r])
        P.op("dve", lambda g: g.bn_aggr(out=st.t[:, 0:2], in_=st.t[:, 16:28]), reads=[st.r], writes=[st.r])
        P.op("act", lambda g: g.activation(out=st.t[:, 2:3], in_=st.t[:, 1:2], func=AF.Ln, bias=self.epsb.t[:, 0:1], scale=1.0), reads=[st.r, self.epsb.r], writes=[st.r])
        P.op("act", lambda g: g.activation(out=st.t[:, 8:9], in_=st.t[:, 2:3], func=AF.Exp, scale=-0.5), reads=[st.r], writes=[st.r])
        P.op("dve", lambda g: g.tensor_scalar(out=st.t[:, 9:10], in0=st.t[:, 0:1], scalar1=-1.0, scalar2=st.t[:, 8:9], op0=ALU.mult, op1=ALU.mult),
             reads=[st.r], writes=[st.r])

    def transpose8(self, src, src_tile, bank, dst, dst_ap3):
        P = self.P
        pbb = self.pb[bank]
        pv = self.pbf(bank)
        for kc in range(8):
            P.op("pe", lambda g, kc=kc: g.transpose(out=pv[:, kc * 128:(kc + 1) * 128], in_=src_tile[:, kc * 128:(kc + 1) * 128], identity=self.ident.t[:]),
                 reads=[src.r, self.ident.r], writes=[pbb.r])
        self.evac(dst, dst_ap3, pbb, pv.rearrange("p (k t) -> p k t", k=8))

    def phase_inproj(self, l, xin):
        nc, P = self.nc, self.P
        self.mark()
        NW = FM_COLS + V_COLS + 16
        W = self.sb([128, 8, NW], BF16, "win")
        wb, wap = self.wsrc("w_in", l)
        wsrc = wap.rearrange("(kc p) n -> p kc n", p=128)
        for kc in range(8):
            self.ld(W, W.t[:, kc, :], wb, wsrc[:, kc, :], "win", q=self.wq)
        xr = Ring([self.sb([128, D], F32, "x") for _ in range(3)])
        xn = Ring([self.sb([128, D], F32, "xn") for _ in range(2)])
        hr = Ring([self.sb([128, D], BF16, "h") for _ in range(2)])
        hTr = Ring([self.sb([128, 8, 512], BF16, "hT") for _ in range(2)])
        fst = Ring([self.sb([128, 512], BF16, "fst") for _ in range(3)])
        vst = Ring([self.sb([128, V_COLS], BF16, "vst") for _ in range(2)])
        wst = Ring([self.sb([128, 16], F32, "wst") for _ in range(2)])
        stt = Ring([self.sb([128, 32], F32, "st") for _ in range(3)])
        xsrc = xin.t.ap().rearrange("(n p) d -> n p d", p=128)
        mod = self.mod
        bank = 0
        for I in range(8):
            hT = hTr.next()
            for j in range(4):
                tb = I * 4 + j
                x = xr.next()
                self.ld(x, x.t[:], xin, xsrc[tb], "x%d" % (tb % 3))
                st = stt.next()
                self.ln_rows(x, x.t[:], st)
                n_ = xn.next()
                P.op("act", lambda g, x=x, n_=n_, st=st: g.activation(out=n_.t[:], in_=x.t[:], func=AF.Identity, bias=st.t[:, 9:10], scale=st.t[:, 8:9]),
                     reads=[x.r, st.r], writes=[n_.r])
                P.op("dve", lambda g, n_=n_: g.tensor_tensor(out=n_.t[:], in0=n_.t[:], in1=mod.t[:, D:2 * D], op=ALU.mult), reads=[n_.r, mod.r], writes=[n_.r])
                h = hr.next()
                P.op("dve", lambda g, n_=n_, h=h: g.tensor_tensor(out=h.t[:], in0=n_.t[:], in1=mod.t[:, 0:D], op=ALU.add), reads=[n_.r, mod.r], writes=[h.r])
                self.transpose8(h, h.t, 6 + (tb % 2), hT, hT.t[:, :, j * 128:(j + 1) * 128])
            for c in range(25):
                ncol = 128 if c < 24 else 64
                pb = self.pb[bank % 4]
                bank += 1
                for kc in range(8):
                    P.op("pe", lambda g, pb=pb, kc=kc, c=c, ncol=ncol, hT=hT: g.matmul(pb.t[0:ncol, :], lhsT=W.t[:, kc, c * 128:c * 128 + ncol], rhs=hT.t[:, kc, :], start=(kc == 0), stop=(kc == 7)),
                         reads=[W.r, hT.r], writes=[pb.r])
                f = fst.next()
                self.evac(f, f.t[0:ncol, :], pb, pb.t[0:ncol, :])
                self.st(self.QT, self.QT.t.ap()[c * 128:c * 128 + ncol, I * 512:(I + 1) * 512], f, f.t[0:ncol, :], "qtst")
            for j in range(4):
                tb = I * 4 + j
                v = vst.next()
                for vc in range(2):
                    pb = self.pb[4 + vc]
                    for kc in range(8):
                        P.op("pe", lambda g, pb=pb, kc=kc, vc=vc, j=j, hT=hT: g.matmul(pb.t[:, :], lhsT=hT.t[:, kc, j * 128:(j + 1) * 128], rhs=W.t[:, kc, FM_COLS + vc * 512:FM_COLS + (vc + 1) * 512], start=(kc == 0), stop=(kc == 7)),
                             reads=[W.r, hT.r], writes=[pb.r])
                    self.evac(v, v.t[:, vc * 512:(vc + 1) * 512], pb, pb.t[:, :])
                self.st(self.VT, self.VT.t.ap()[tb * 128:(tb + 1) * 128, :], v, v.t[:], "vtst")
                pb = self.pb[bank % 4]
                bank += 1
                for kc in range(8):
                    P.op("pe", lambda g, pb=pb, kc=kc, j=j, hT=hT: g.matmul(pb.t[:, 0:16], lhsT=hT.t[:, kc, j * 128:(j + 1) * 128], rhs=W.t[:, kc, FM_COLS + V_COLS:NW], start=(kc == 0), stop=(kc == 7)),
                         reads=[W.r, hT.r], writes=[pb.r])
                w = wst.next()
                self.evac(w, w.t[:], pb, pb.t[:, 0:16])
                self.st(self.WX, self.WX.t.ap()[tb * 128:(tb + 1) * 128, :], w, w.t[:], "wxst")
        self.release()

    def phase_bias(self):
        nc, P = self.nc, self.P
        self.mark()
        rb = self.sb([32, 12], F32, "rb")
        self.ld(rb, rb.t[:], self.rel_bias, self.rel_bias.t.ap(), "rb")
        eb = self.sb([32, 12], F32, "eb")
        P.op("act", lambda g: g.activation(out=eb.t[:], in_=rb.t[:], func=AF.Exp), reads=[rb.r], writes=[eb.r])
        ohb = self.sb([32, 2304], F32, "ohb")
        self.ld(ohb, ohb.t[:], self.ohb, self.ohb.t.ap(), "ohb")
        ohc = self.sb([32, 384], F32, "ohc")
        self.ld(ohc, ohc.t[:], self.ohc, self.ohc.t.ap(), "ohc")
        mv = self.sb([12, 2304], F32, "mv")
        P.op("dve", lambda g: g.memset(mv.t[:], 0.0), writes=[mv.r])
        k = 0
        for c0 in range(0, 2304, 512):
            w = min(512, 2304 - c0)
            pb = self.pb[k % 4]; k += 1
            P.op("pe", lambda g, pb=pb, c0=c0, w=w: g.matmul(pb.t[0:4, 0:w], lhsT=eb.t[:, 0:4], rhs=ohb.t[:, c0:c0 + w], start=True, stop=True), reads=[eb.r, ohb.r], writes=[pb.r])
            P.op("dve", lambda g, pb=pb, c0=c0, w=w: g.tensor_copy(out=mv.t[0:4, c0:c0 + w], in_=pb.t[0:4, 0:w]), reads=[pb.r], writes=[mv.r])
        pb = self.pb[k % 4]
        P.op("pe", lambda g: g.matmul(pb.t[0:12, 0:384], lhsT=eb.t[:, 0:12], rhs=ohc.t[:, 0:384], start=True, stop=True), reads=[eb.r, ohc.r], writes=[pb.r])
        mv2 = self.sb([12, 384], F32, "mv2")
        P.op("dve", lambda g: g.tensor_copy(out=mv2.t[:], in_=pb.t[0:12, 0:384]), reads=[pb.r], writes=[mv2.r])
        self.st(self.MV, self.MV.t.ap()[0:4, :], mv, mv.t[0:4, :], "mvst")
        self.st(self.MV, self.MV.t.ap()[4:12, 0:384], mv2, mv2.t[4:12, :], "mvst")
        self.release()

    def hankel(self, dst, dst_ap, row, width, q="pool"):
        base = self.MV.t.ap()
        src = bass.AP(tensor=base.tensor, offset=row * 2304, ap=[[1, 128], [1, width]])
        return self.ld(dst, dst_ap, self.MV, src, "hk", q=q)

    def load_qkv(self, qoff, koff, voff, dmode=False):
        P = self.P
        if dmode:
            Q = self.sb([64, 4, T], BF16, "Q"); Kt = self.sb([64, 4, T], BF16, "K")
            for h in range(4):
                self.ld(Q, Q.t[:, h, :], self.QT, self.QT.t.ap()[qoff + h * 64:qoff + (h + 1) * 64, :], "ldq")
                self.ld(Kt, Kt.t[:, h, :], self.QT, self.QT.t.ap()[koff + h * 64:koff + (h + 1) * 64, :], "ldk")
        else:
            Q = self.sb([128, 2, T], BF16, "Q"); Kt = self.sb([128, 2, T], BF16, "K")
            for j in range(2):
                self.ld(Q, Q.t[:, j, :], self.QT, self.QT.t.ap()[qoff + j * 128:qoff + (j + 1) * 128, :], "ldq")
                self.ld(Kt, Kt.t[:, j, :], self.QT, self.QT.t.ap()[koff + j * 128:koff + (j + 1) * 128, :], "ldk")
        Vr = self.sb([128, NB, 256], BF16, "Vr")
        self.ld(Vr, Vr.t[:], self.VT, self.VT.t.ap().rearrange("(n p) c -> p n c", p=128)[:, :, voff:voff + 256], "ldv")
        V = self.sb([128, NB, 4, 65], BF16, "V")
        P.op("pool", lambda g: g.memset(V.t[:], 1.0), writes=[V.r])
        for h in range(4):
            P.op("pool", lambda g, h=h: g.tensor_copy(out=V.t[:, :, h, 0:64], in_=Vr.t[:, :, h * 64:(h + 1) * 64]), reads=[Vr.r, V.r], writes=[V.r])
        return Q, Kt, V

    def qk_ap(self, Q, h, lo, hi, dmode=False, m=0):
        if dmode:
            return Q.t[m * 32:(m + 1) * 32, h, lo:hi]
        return Q.t[(h % 2) * 64:(h % 2) * 64 + 64, h // 2, lo:hi]

    def pv_rows(self, Pr, pr_lo, lo_kb, qb, V, h, obank, ocol, ptr, first_start=True):
        P = self.P
        kbs = list(range(lo_kb, qb + 1))
        pend = None

        def emit_pv(grp, pt):
            for i, kb in enumerate(grp):
                P.op("pe", lambda g, i=i, kb=kb, pt=pt: g.matmul(obank.t[:, ocol:ocol + 65], lhsT=pt.t[:, i * 128:(i + 1) * 128], rhs=V.t[:, kb, h, :], start=(kb == lo_kb), stop=(kb == qb)),
                     reads=[pt.r, V.r], writes=[obank.r])

        for g0 in range(0, len(kbs), 4):
            grp = kbs[g0:g0 + 4]
            self._tb = getattr(self, "_tb", 0) + 1
            bank = 4 + (self._tb % 2)
            pbb = self.pb[bank]
            pv = self.pbf(bank)
            for i, kb in enumerate(grp):
                c = (kb - lo_kb) * 128 + pr_lo
                P.op("pe", lambda g, i=i, c=c, pv=pv: g.transpose(out=pv[:, i * 128:(i + 1) * 128], in_=Pr.t[:, c:c + 128], identity=self.ident.t[:]),
                     reads=[Pr.r, self.ident.r], writes=[pbb.r])
            pt = ptr.next()
            ng = len(grp)
            self.evac(pt, pt.t[:, 0:ng * 128], pbb, pv[:, 0:ng * 128])
            if pend is not None:
                emit_pv(*pend)
            pend = (grp, pt)
        emit_pv(*pend)

    def phase_attA(self, l):
        nc, P = self.nc, self.P
        self.mark()
        Q, Kt, V = self.load_qkv(0, 256, 0)
        SPr = Ring([self.sb([128, T], F32, "SP") for _ in range(2)])
        TMr = Ring([self.sb([128, T], F32, "TM") for _ in range(1)])
        Fr = Ring([self.sb([128, T], F32, "F") for _ in range(1)])
        Prr = Ring([self.sb([128, T], BF16, "Pr") for _ in range(2)])
        ptr = Ring([self.sb([128, 512], BF16, "pt") for _ in range(3)])
        ostr = Ring([self.sb([128, 256], BF16, "ost") for _ in range(2)])
        smr = Ring([self.sb([128, 4], F32, "sm") for _ in range(4)])
        sb_i = 0
        for qb in range(NB):
            n = (qb + 1) * 128
            obank = self.pb[6 + (qb % 2)]
            for h in range(4):
                SP, TM, F, Pr = SPr.next(), TMr.next(), Fr.next(), Prr.next()
                for c0 in range(0, n, 512):
                    c1 = min(n, c0 + 512); w = c1 - c0
                    pb = self.pb[sb_i % 4]; sb_i += 1
                    P.op("pe", lambda g, pb=pb, c0=c0, c1=c1, w=w, h=h, qb=qb: g.matmul(pb.t[:, 0:w], lhsT=self.qk_ap(Q, h, qb * 128, (qb + 1) * 128), rhs=self.qk_ap(Kt, h, c0, c1), start=True, stop=True),
                         reads=[Q.r, Kt.r], writes=[pb.r])
                    P.op("act", lambda g, pb=pb, c0=c0, c1=c1, w=w, F=F: g.activation(out=F.t[:, c0:c1], in_=pb.t[:, 0:w], func=AF.Exp, scale=0.125), reads=[pb.r], writes=[F.r])
                    P.op("act", lambda g, c0=c0, c1=c1, F=F, SP=SP: g.activation(out=SP.t[:, c0:c1], in_=F.t[:, c0:c1], func=AF.Ln, bias=self.oneb.t[:, 0:1], scale=1.0), reads=[F.r, self.oneb.r], writes=[SP.r])
                    P.op("dve", lambda g, pb=pb, c0=c0, c1=c1, w=w, SP=SP, TM=TM: g.scalar_tensor_tensor(out=TM.t[:, c0:c1], in0=pb.t[:, 0:w], scalar=0.125, in1=SP.t[:, c0:c1], op0=ALU.mult, op1=ALU.subtract),
                         reads=[pb.r, SP.r], writes=[TM.r])
                P.op("dve", lambda g, SP=SP, n=n: g.tensor_tensor(out=SP.t[:, n - 128:n], in0=SP.t[:, n - 128:n], in1=self.tri_lt32.t[:], op=ALU.mult), reads=[SP.r, self.tri_lt32.r], writes=[SP.r])
                P.op("dve", lambda g, SP=SP, F=F, n=n: g.tensor_tensor_scan(out=F.t[:, 0:n], data0=SP.t[:, 0:n], data1=SP.t[:, 0:n], initial=0.0, op0=ALU.add, op1=ALU.max), reads=[SP.r, F.r], writes=[F.r])
                sm = smr.next()
                P.op("dve", lambda g, F=F, sm=sm, n=n: g.tensor_scalar(out=sm.t[:, 0:1], in0=F.t[:, n - 1:n], scalar1=-1.0, scalar2=None, op0=ALU.mult), reads=[F.r], writes=[sm.r])
                P.op("dve", lambda g, F=F, TM=TM, n=n: g.tensor_tensor(out=TM.t[:, 0:n], in0=TM.t[:, 0:n], in1=F.t[:, 0:n], op=ALU.add), reads=[F.r, TM.r], writes=[TM.r])
                P.op("act", lambda g, TM=TM, Pr=Pr, sm=sm, n=n: g.activation(out=Pr.t[:, 0:n], in_=TM.t[:, 0:n], func=AF.Exp, bias=sm.t[:, 0:1], scale=1.0), reads=[TM.r, sm.r], writes=[Pr.r])
                P.op("pool", lambda g, Pr=Pr, n=n: g.tensor_tensor(out=Pr.t[:, n - 128:n], in0=Pr.t[:, n - 128:n], in1=self.tri_lt.t[:], op=ALU.mult), reads=[Pr.r, self.tri_lt.r], writes=[Pr.r])
                self.pv_rows(Pr, 0, 0, qb, V, h, obank, h * 65, ptr)
            ost = ostr.next()
            self.evac(ost, ost.t[:].rearrange("p (h d) -> p h d", h=4), obank, obank.t[:, 0:260].rearrange("p (h d) -> p h d", h=4)[:, :, 0:64])
            self.st(self.MT, self.MT.t.ap()[qb * 128:(qb + 1) * 128, 0:256], ost, ost.t[:], "mtst")
        self.release()

    def softmax_finish(self, obank, ostr, qb, mcol, smr):
        P = self.P
        sm = smr.next()
        ov = obank.t[:, 0:260].rearrange("p (h d) -> p h d", h=4)
        P.op("dve", lambda g: g.reciprocal(out=sm.t[:, 0:4], in_=ov[:, :, 64]), reads=[obank.r], writes=[sm.r])
        ost = ostr.next()
        for h in range(4):
            P.op("dve", lambda g, h=h: g.tensor_scalar(out=ost.t[:, h * 64:(h + 1) * 64], in0=obank.t[:, h * 65:h * 65 + 64], scalar1=sm.t[:, h:h + 1], scalar2=None, op0=ALU.mult),
                 reads=[obank.r, sm.r], writes=[ost.r])
        self.st(self.MT, self.MT.t.ap()[qb * 128:(qb + 1) * 128, mcol:mcol + 256], ost, ost.t[:], "mtst")

    def phase_attB(self, l):
        nc, P = self.nc, self.P
        self.mark()
        Q, Kt, V = self.load_qkv(512, 768, 256)
        Hk = self.sb([128, 4, 2176], BF16, "HkB")
        for h in range(4):
            self.hankel(Hk, Hk.t[:, h, :], h, 2176)
        Prr = Ring([self.sb([128, 2176], BF16, "Pr") for _ in range(3)])
        ptr = Ring([self.sb([128, 512], BF16, "pt") for _ in range(3)])
        ostr = Ring([self.sb([128, 256], BF16, "ost") for _ in range(2)])
        smr = Ring([self.sb([128, 4], F32, "sm") for _ in range(3)])
        sb_i = 0
        for qb in range(NB):
            n = (qb + 1) * 128
            lo_kb = max(0, qb - 16)
            lo = lo_kb * 128
            cnt = n - lo
            cplo = lo - 128 * qb + 2048
            a = 2175 - cplo - cnt + 1
            obank = self.pb[6 + (qb % 2)]
            for h in range(4):
                Pr = Prr.next()
                for c0 in range(lo, n, self.SCH):
                    c1 = min(n, c0 + self.SCH); w = c1 - c0
                    pb = self.pb[sb_i % 4]; sb_i += 1
                    P.op("pe", lambda g, pb=pb, c0=c0, c1=c1, w=w, h=h, qb=qb: g.matmul(pb.t[:, 0:w], lhsT=self.qk_ap(Q, h, qb * 128, (qb + 1) * 128), rhs=self.qk_ap(Kt, h, c0, c1), start=True, stop=True),
                         reads=[Q.r, Kt.r], writes=[pb.r])
                    P.op("act", lambda g, pb=pb, c0=c0, c1=c1, w=w, Pr=Pr, lo=lo: g.activation(out=Pr.t[:, c0 - lo:c1 - lo], in_=pb.t[:, 0:w], func=AF.Exp, scale=0.125), reads=[pb.r], writes=[Pr.r])
                P.op("dve", lambda g, Pr=Pr, h=h, a=a, cnt=cnt: g.tensor_tensor(out=Pr.t[:, 0:cnt], in0=Pr.t[:, 0:cnt], in1=rev_ap(Hk.t[:, h, a:a + cnt], cnt), op=ALU.mult),
                     reads=[Pr.r, Hk.r], writes=[Pr.r])
                self.pv_rows(Pr, 0, lo_kb, qb, V, h, obank, h * 65, ptr)
            self.softmax_finish(obank, ostr, qb, 256, smr)
        self.release()

    def cd_row(self, Q, Kt, h, qb, Pr, HkCD, hk_idx, b31, bcol, scale, sb_i, dmode=False, m=0):
        P = self.P
        n = (qb + 1) * 128
        near0 = max(0, n - 256)
        for c0 in range(0, n, 512):
            c1 = min(n, c0 + 512); w = c1 - c0
            pb = self.pb[sb_i[0] % self.nsb]; sb_i[0] += 1
            P.op("pe", lambda g, pb=pb, c0=c0, c1=c1, w=w: g.matmul(pb.t[:, 0:w], lhsT=self.qk_ap(Q, h, qb * 128, (qb + 1) * 128, dmode, m), rhs=self.qk_ap(Kt, h, c0, c1, dmode, m), start=True, stop=True),
                 reads=[Q.r, Kt.r], writes=[pb.r])
            f1 = min(c1, near0)
            if f1 > c0:
                P.op("act", lambda g, pb=pb, c0=c0, f1=f1: g.activation(out=Pr.t[:, c0:f1], in_=pb.t[:, 0:f1 - c0], func=AF.Exp, bias=b31.t[:, bcol:bcol + 1], scale=scale), reads=[pb.r, b31.r], writes=[Pr.r])
            n0 = max(c0, near0)
            if c1 > n0:
                P.op("act", lambda g, pb=pb, c0=c0, c1=c1, n0=n0: g.activation(out=Pr.t[:, n0:c1], in_=pb.t[:, n0 - c0:c1 - c0], func=AF.Exp, scale=scale), reads=[pb.r], writes=[Pr.r])
        nn = n - near0
        P.op("dve", lambda g: g.tensor_tensor(out=Pr.t[:, near0:n], in0=Pr.t[:, near0:n], in1=rev_ap(HkCD.t[:, hk_idx, 0:nn], nn), op=ALU.mult), reads=[Pr.r, HkCD.r], writes=[Pr.r])

    def load_cd_bias(self, row0):
        HkCD = self.sb([128, 4, 256], BF16, "HkCD")
        for h in range(4):
            self.hankel(HkCD, HkCD.t[:, h, :], row0 + h, 256)
        b31 = self.sb([128, 12], F32, "b31")
        self.ld(b31, b31.t[:], self.rel_bias, self.rel_bias.t.ap()[31:32, :].partition_broadcast(128), "b31")
        return HkCD, b31

    def phase_attD(self, l):
        nc, P = self.nc, self.P
        import math
        lambda_init = 0.8 - 0.6 * math.exp(-0.3 * l)
        self.mark()
        Q, Kt, V = self.load_qkv(2560, 2816, 768, dmode=True)
        HkCD, b31 = self.load_cd_bias(8)
        dl = self.sb([128, 128], F32, "dl")
        self.ld(dl, dl.t[:], self.diff_lam, self.diff_lam.t.ap()[l:l + 1, :].partition_broadcast(128), "dl")
        lam = self.sb([128, 8], F32, "lam")
        jk = self.sb([128, 32], F32, "jk")
        P.op("dve", lambda g: g.scalar_tensor_tensor(out=jk.t[:], in0=dl.t[:, 0:32], scalar=1.0, in1=dl.t[:, 32:64], op0=ALU.mult, op1=ALU.mult, accum_out=lam.t[:, 0:1]), reads=[dl.r], writes=[jk.r, lam.r])
        P.op("dve", lambda g: g.scalar_tensor_tensor(out=jk.t[:], in0=dl.t[:, 64:96], scalar=1.0, in1=dl.t[:, 96:128], op0=ALU.mult, op1=ALU.mult, accum_out=lam.t[:, 1:2]), reads=[dl.r, jk.r, lam.r], writes=[jk.r, lam.r])
        P.op("act", lambda g: g.activation(out=lam.t[:, 2:4], in_=lam.t[:, 0:2], func=AF.Exp), reads=[lam.r], writes=[lam.r])
        P.op("dve", lambda g: g.tensor_tensor(out=lam.t[:, 4:5], in0=lam.t[:, 2:3], in1=lam.t[:, 3:4], op=ALU.subtract), reads=[lam.r], writes=[lam.r])
        P.op("dve", lambda g: g.tensor_scalar(out=lam.t[:, 5:6], in0=lam.t[:, 4:5], scalar1=lambda_init, scalar2=-1.0, op0=ALU.add, op1=ALU.mult), reads=[lam.r], writes=[lam.r])
        gbc = self.sb([128, 64], F32, "gbc")
        self.ld(gbc, gbc.t[:], self.diff_g, self.diff_g.t.ap()[l:l + 1, :].partition_broadcast(128), "gbc")
        P.op("dve", lambda g: g.tensor_scalar(out=gbc.t[:], in0=gbc.t[:], scalar1=1.0 - lambda_init, scalar2=None, op0=ALU.mult), reads=[gbc.r], writes=[gbc.r])
        Prr = Ring([self.sb([128, T], BF16, "Pr") for _ in range(3)])
        ptr = Ring([self.sb([128, 512], BF16, "pt") for _ in range(3)])
        ostr = Ring([self.sb([128, 256], BF16, "ost") for _ in range(2)])
        smr = Ring([self.sb([128, 16], F32, "sm") for _ in range(4)])
        o1r = Ring([self.sb([128, 64], F32, "o1") for _ in range(3)])
        sb_i = [0]
        scale = 32 ** -0.5
        for qb in range(NB):
            ost = ostr.next()
            for h in range(4):
                obank = self.pb[6 + (h % 2)]
                for m in range(2):
                    Pr = Prr.next()
                    self.cd_row(Q, Kt, h, qb, Pr, HkCD, h, b31, 8 + h, scale, sb_i, dmode=True, m=m)
                    self.pv_rows(Pr, 0, 0, qb, V, h, obank, m * 65, ptr)
                sm = smr.next(); o1 = o1r.next(); o2 = o1r.next()
                P.op("dve", lambda g, obank=obank, sm=sm: g.reciprocal(out=sm.t[:, 0:2], in_=obank.t[:, 0:130].rearrange("p (m d) -> p m d", m=2)[:, :, 64]), reads=[obank.r], writes=[sm.r])
                P.op("dve", lambda g, sm=sm: g.tensor_tensor(out=sm.t[:, 2:3], in0=sm.t[:, 1:2], in1=lam.t[:, 5:6], op=ALU.mult), reads=[sm.r, lam.r], writes=[sm.r])
                P.op("dve", lambda g, obank=obank, sm=sm, o1=o1: g.tensor_scalar(out=o1.t[:], in0=obank.t[:, 0:64], scalar1=sm.t[:, 0:1], scalar2=None, op0=ALU.mult), reads=[obank.r, sm.r], writes=[o1.r])
                P.op("dve", lambda g, obank=obank, sm=sm, o1=o1, o2=o2: g.scalar_tensor_tensor(out=o2.t[:], in0=obank.t[:, 65:129], scalar=sm.t[:, 2:3], in1=o1.t[:], op0=ALU.mult, op1=ALU.add), reads=[obank.r, sm.r, o1.r], writes=[o2.r])
                P.op("dve", lambda g, sm=sm, o1=o1, o2=o2: g.scalar_tensor_tensor(out=o1.t[:], in0=o2.t[:], scalar=1.0, in1=o2.t[:], op0=ALU.mult, op1=ALU.mult, accum_out=sm.t[:, 3:4]), reads=[o2.r, o1.r, sm.r], writes=[o1.r, sm.r])
                P.op("act", lambda g, sm=sm: g.activation(out=sm.t[:, 4:5], in_=sm.t[:, 3:4], func=AF.Ln, bias=self.epsb.t[:, 0:1], scale=1.0 / 64), reads=[sm.r, self.epsb.r], writes=[sm.r])
                P.op("act", lambda g, sm=sm: g.activation(out=sm.t[:, 5:6], in_=sm.t[:, 4:5], func=AF.Exp, scale=-0.5), reads=[sm.r], writes=[sm.r])
                P.op("dve", lambda g, sm=sm, o2=o2, h=h, ost=ost: g.scalar_tensor_tensor(out=ost.t[:, h * 64:(h + 1) * 64], in0=o2.t[:], scalar=sm.t[:, 5:6], in1=gbc.t[:], op0=ALU.mult, op1=ALU.mult), reads=[o2.r, sm.r, gbc.r], writes=[ost.r])
            self.st(self.MT, self.MT.t.ap()[qb * 128:(qb + 1) * 128, 768:1024], ost, ost.t[:], "mtst")
        self.release()

    def phase_attC(self, l):
        nc, P = self.nc, self.P
        self.mark()
        self.nsb = 3
        Q, Kt, V = self.load_qkv(1024, 1280, 512)
        HkCD, b31 = self.load_cd_bias(4)
        KX = self.sb([128, T], BF16, "KX")
        for half in range(2):
            self.ld(KX, KX.t[half * 64:(half + 1) * 64, :], self.QT, self.QT.t.ap()[3072:3136, :], "ldkx")
        qxr = Ring([self.sb([128, 8, 128], BF16, "qx") for _ in range(2)])
        wxr = Ring([self.sb([128, 16], F32, "wx") for _ in range(2)])
        dgr = Ring([self.sb([128, 16, 128], BF16, "dg") for _ in range(2)])
        Rr = Ring([self.sb([128, 512], BF16, "R") for _ in range(4)])
        SC = self.sb([128, T], F32, "SC")
        junk = self.sb([128, T], BF16, "junk")
        mskr = Ring([self.sb([128, T], BF16, "msk") for _ in range(2)])
        bsr = Ring([self.sb([128, 64], F32, "bs") for _ in range(2)])
        Prr = Ring([self.sb([128, T], BF16, "Pr") for _ in range(2)])
        ptr = Ring([self.sb([128, 512], BF16, "pt") for _ in range(3)])
        ostr = Ring([self.sb([128, 256], BF16, "ost") for _ in range(2)])
        smr = Ring([self.sb([128, 4], F32, "sm") for _ in range(3)])
        sb_i = [0]
        qxsrc = self.QT.t.ap()[1536:2560, :].rearrange("(j p) t -> p j t", p=128)
        NIT = 22
        SCr = Ring([SC, self.sb([128, T], F32, "SC2")])
        scs = {}

        def indexer(qb):
            n = (qb + 1) * 128
            qx = qxr.next(); wx = wxr.next(); dg = dgr.next()
            SCq = SCr.next()
            scs[qb] = SCq
            self.ld(qx, qx.t[:], self.QT, qxsrc[:, :, qb * 128:(qb + 1) * 128], "ldqx%d" % (qb % 2))
            self.ld(wx, wx.t[:], self.WX, self.WX.t.ap()[qb * 128:(qb + 1) * 128, :], "ldwx%d" % (qb % 2))
            for hh in range(16):
                P.op("pool", lambda g, hh=hh, dg=dg, wx=wx: g.tensor_scalar(out=dg.t[:, hh, :], in0=self.ident.t[:], scalar1=wx.t[:, hh:hh + 1], scalar2=0.25, op0=ALU.mult, op1=ALU.mult),
                     reads=[self.ident.r, wx.r], writes=[dg.r])
            scb = self.pb[3]
            for c0 in range(0, n, 512):
                c1 = min(n, c0 + 512); w = c1 - c0
                for hh in range(16):
                    j, half = hh // 2, hh % 2
                    pb = self.pb[sb_i[0] % 3]; sb_i[0] += 1
                    P.op("pe", lambda g, pb=pb, j=j, half=half, c0=c0, c1=c1, w=w, qx=qx: g.matmul(pb.t[:, 0:w], lhsT=qx.t[half * 64:(half + 1) * 64, j, :], rhs=KX.t[half * 64:(half + 1) * 64, c0:c1], start=True, stop=True),
                         reads=[qx.r, KX.r], writes=[pb.r])
                    R = Rr.next()
                    P.op("act", lambda g, pb=pb, w=w, R=R: g.activation(out=R.t[:, 0:w], in_=pb.t[:, 0:w], func=AF.Relu, scale=0.125), reads=[pb.r], writes=[R.r])
                    P.op("pe", lambda g, hh=hh, w=w, R=R, dg=dg: g.matmul(scb.t[:, 0:w], lhsT=dg.t[:, hh, :], rhs=R.t[:, 0:w], start=(hh == 0), stop=(hh == 15)),
                         reads=[dg.r, R.r], writes=[scb.r])
                self.evac(SCq, SCq.t[:, c0:c1], scb, scb.t[:, 0:w], eng="act")

        def rest(qb):
            n = (qb + 1) * 128
            SC = scs.pop(qb)
            msk = mskr.next(); bs = bsr.next()
            if qb >= 2:
                P.op("dve", lambda g, bs=bs, n=n: g.tensor_reduce(out=bs.t[:, 0:1], in_=SC.t[:, 0:n], axis=AX.X, op=ALU.max), reads=[SC.r], writes=[bs.r])
                P.op("dve", lambda g, bs=bs, n=n: g.tensor_reduce(out=bs.t[:, 1:2], in_=SC.t[:, 0:n], axis=AX.X, op=ALU.min), reads=[SC.r, bs.r], writes=[bs.r])
            P.op("dve", lambda g, n=n: g.tensor_tensor(out=SC.t[:, n - 128:n], in0=SC.t[:, n - 128:n], in1=self.negm.t[:], op=ALU.add), reads=[SC.r, self.negm.r], writes=[SC.r])
            if qb >= 2:
                P.op("dve", lambda g, bs=bs: g.tensor_tensor(out=bs.t[:, 2:3], in0=bs.t[:, 0:1], in1=bs.t[:, 1:2], op=ALU.subtract), reads=[bs.r], writes=[bs.r])
                P.op("dve", lambda g, bs=bs: g.tensor_scalar(out=bs.t[:, 2:3], in0=bs.t[:, 2:3], scalar1=1.0001, scalar2=1e-6, op0=ALU.mult, op1=ALU.add), reads=[bs.r], writes=[bs.r])
                P.op("dve", lambda g, bs=bs: g.tensor_scalar(out=bs.t[:, 8:8 + NIT], in0=self.pow2.t[:, 0:NIT], scalar1=bs.t[:, 2:3], scalar2=None, op0=ALU.mult), reads=[bs.r, self.pow2.r], writes=[bs.r])
                for it in range(NIT):
                    P.op("dve", lambda g, bs=bs, it=it: g.tensor_tensor(out=bs.t[:, 3:4], in0=bs.t[:, 1:2], in1=bs.t[:, 8 + it:9 + it], op=ALU.add), reads=[bs.r], writes=[bs.r])
                    P.op("dve", lambda g, bs=bs, n=n: g.tensor_scalar(out=junk.t[:, 0:n], in0=SC.t[:, 0:n], scalar1=bs.t[:, 3:4], scalar2=None, op0=ALU.is_ge, op1=ALU.add, accum_out=bs.t[:, 4:5]),
                         reads=[SC.r, bs.r, junk.r], writes=[junk.r, bs.r])
                    P.op("dve", lambda g, bs=bs, it=it: g.tensor_scalar(out=bs.t[:, 5:6], in0=bs.t[:, 4:5], scalar1=255.5, scalar2=bs.t[:, 8 + it:9 + it], op0=ALU.is_ge, op1=ALU.mult), reads=[bs.r], writes=[bs.r])
                    P.op("dve", lambda g, bs=bs: g.tensor_tensor(out=bs.t[:, 1:2], in0=bs.t[:, 1:2], in1=bs.t[:, 5:6], op=ALU.add), reads=[bs.r], writes=[bs.r])
                P.op("dve", lambda g, bs=bs, msk=msk, n=n: g.tensor_scalar(out=msk.t[:, 0:n], in0=SC.t[:, 0:n], scalar1=bs.t[:, 1:2], scalar2=None, op0=ALU.is_ge), reads=[SC.r, bs.r], writes=[msk.r])
            else:
                P.op("dve", lambda g, msk=msk, n=n: g.tensor_scalar(out=msk.t[:, 0:n], in0=SC.t[:, 0:n], scalar1=-1e29, scalar2=None, op0=ALU.is_ge), reads=[SC.r], writes=[msk.r])
            obank = self.pb[6 + (qb % 2)]
            for h in range(4):
                Pr = Prr.next()
                self.cd_row(Q, Kt, h, qb, Pr, HkCD, h, b31, 4 + h, 0.125, sb_i)
                P.op("dve", lambda g, Pr=Pr, msk=msk, n=n: g.tensor_tensor(out=Pr.t[:, 0:n], in0=Pr.t[:, 0:n], in1=msk.t[:, 0:n], op=ALU.mult), reads=[Pr.r, msk.r], writes=[Pr.r])
                self.pv_rows(Pr, 0, 0, qb, V, h, obank, h * 65, ptr)
            self.softmax_finish(obank, ostr, qb, 512, smr)

        indexer(0)
        for qb in range(NB):
            if qb + 1 < NB:
                indexer(qb + 1)
            rest(qb)
        self.nsb = 4
        self.release()

    def load_bc(self, src, ap_row, key):
        t = self.sb([128, D], F32, key)
        self.ld(t, t.t[:], src, ap_row.partition_broadcast(128), key)
        return t

    def phase_outproj(self, l, xin):
        nc, P = self.nc, self.P
        self.mark()
        mod = self.mod
        Wo = self.sb([128, 8, D], BF16, "wo")
        wb, wap = self.wsrc("w_out", l)
        self.ld(Wo, Wo.t[:], wb, wap.rearrange("(kc p) n -> p kc n", p=128), "wo", q=self.wq)
        Wr = self.sb([128, 8, NE], BF16, "wr")
        self.ld(Wr, Wr.t[:], self.w_router, self.w_router.t.ap()[l].rearrange("(kc p) n -> p kc n", p=128), "wr", q="pool")
        brt = self.sb([128, NE], F32, "brt")
        self.ld(brt, brt.t[:], self.b_router, self.b_router.t.ap()[l:l + 1, :].partition_broadcast(128), "brt")
        lg = self.load_bc(self.ln_g, self.ln_g.t.ap()[l, 0:1, :], "lng")
        lb = self.load_bc(self.ln_b, self.ln_b.t.ap()[l, 0:1, :], "lnb")
        selsum = self.sb([128, NE], F32, "selsum")
        selsum_bf = self.sb([128, NE], BF16, "selsum_bf")
        P.op("dve", lambda g: g.memset(selsum.t[:], 0.0), writes=[selsum.r])
        P.op("dve", lambda g: g.memset(selsum_bf.t[:], 0.0), writes=[selsum_bf.r])
        mr = Ring([self.sb([128, D], BF16, "m") for _ in range(2)])
        mTr = Ring([self.sb([128, 8, 128], BF16, "mT") for _ in range(2)])
        xr = Ring([self.sb([128, D], F32, "x") for _ in range(2)])
        zr = Ring([self.sb([128, D], F32, "z") for _ in range(2)])
        x1r = Ring([self.sb([128, D], F32, "x1") for _ in range(2)])
        hnr = Ring([self.sb([128, D], F32, "hn") for _ in range(2)])
        h2r = Ring([self.sb([128, D], BF16, "h2") for _ in range(3)])
        h2Tr = Ring([self.sb([128, 8, 128], BF16, "h2T") for _ in range(2)])
        stt = Ring([self.sb([128, 32], F32, "st") for _ in range(4)])
        rtr = Ring([self.sb([128, 160], F32, "rt") for _ in range(2)])
        selr = Ring([self.sb([128, NE], F32, "sel") for _ in range(2)])
        idr = Ring([self.sb([128, 8], U32, "idu") for _ in range(2)])
        ixr = Ring([self.sb([128, 1], I32, "ix") for _ in range(8)])
        msrc = self.MT.t.ap().rearrange("(n p) d -> n p d", p=128)
        xsrc = xin.t.ap().rearrange("(n p) d -> n p d", p=128)
        x1dst = self.X1.t.ap().rearrange("(n p) d -> n p d", p=128)
        for tb in range(NB):
            m = mr.next(); mT = mTr.next(); x = xr.next(); z = zr.next(); x1 = x1r.next(); hn = hnr.next(); h2 = h2r.next(); h2T = h2Tr.next()
            self.ld(m, m.t[:], self.MT, msrc[tb], "ldm%d" % (tb % 2))
            self.ld(x, x.t[:], xin, xsrc[tb], "ldx%d" % (tb % 2))
            self.transpose8(m, m.t, 4 + (tb % 2), mT, mT.t[:])
            for dc in range(2):
                pb = self.pb[dc]
                for kc in range(8):
                    P.op("pe", lambda g, pb=pb, kc=kc, dc=dc, mT=mT: g.matmul(pb.t[:, :], lhsT=mT.t[:, kc, :], rhs=Wo.t[:, kc, dc * 512:(dc + 1) * 512], start=(kc == 0), stop=(kc == 7)),
                         reads=[mT.r, Wo.r], writes=[pb.r])
                P.op("dve", lambda g, pb=pb, dc=dc, z=z: g.tensor_tensor(out=z.t[:, dc * 512:(dc + 1) * 512], in0=pb.t[:, :], in1=mod.t[:, 2 * D + dc * 512:2 * D + (dc + 1) * 512], op=ALU.mult),
                     reads=[pb.r, mod.r], writes=[z.r])
            P.op("dve", lambda g, x=x, z=z: g.scalar_tensor_tensor(out=z.t[:], in0=x.t[:], scalar=ALPHA, in1=z.t[:], op0=ALU.mult, op1=ALU.add), reads=[x.r, z.r], writes=[z.r])
            st = stt.next()
            self.ln_rows(z, z.t[:], st)
            P.op("act", lambda g, z=z, st=st: g.activation(out=z.t[:], in_=z.t[:], func=AF.Identity, bias=st.t[:, 9:10], scale=st.t[:, 8:9]), reads=[z.r, st.r], writes=[z.r])
            P.op("pool", lambda g, z=z: g.tensor_tensor(out=z.t[:], in0=z.t[:], in1=lg.t[:], op=ALU.mult), reads=[z.r, lg.r], writes=[z.r])
            P.op("dve", lambda g, z=z, x1=x1: g.tensor_tensor(out=x1.t[:], in0=z.t[:], in1=lb.t[:], op=ALU.add), reads=[z.r, lb.r], writes=[x1.r])
            self.st(self.X1, x1dst[tb], x1, x1.t[:], "x1st")
            st2 = stt.next()
            self.ln_rows(x1, x1.t[:], st2)
            P.op("act", lambda g, x1=x1, hn=hn, st2=st2: g.activation(out=hn.t[:], in_=x1.t[:], func=AF.Identity, bias=st2.t[:, 9:10], scale=st2.t[:, 8:9]), reads=[x1.r, st2.r], writes=[hn.r])
            P.op("pool", lambda g, hn=hn: g.tensor_tensor(out=hn.t[:], in0=hn.t[:], in1=mod.t[:, 4 * D:5 * D], op=ALU.mult), reads=[hn.r, mod.r], writes=[hn.r])
            P.op("dve", lambda g, hn=hn, h2=h2: g.tensor_tensor(out=h2.t[:], in0=hn.t[:], in1=mod.t[:, 3 * D:4 * D], op=ALU.add), reads=[hn.r, mod.r], writes=[h2.r])
            self.transpose8(h2, h2.t, 6 + (tb % 2), h2T, h2T.t[:])
            pb = self.pb[2]
            for kc in range(8):
                P.op("pe", lambda g, kc=kc, h2T=h2T: g.matmul(pb.t[:, 0:NE], lhsT=h2T.t[:, kc, :], rhs=Wr.t[:, kc, :], start=(kc == 0), stop=(kc == 7)), reads=[h2T.r, Wr.r], writes=[pb.r])
            rt = rtr.next(); sel = selr.next(); idu = idr.next()
            lgt = rt.t[:, 0:32]; top8 = rt.t[:, 32:40]; ex4 = rt.t[:, 40:44]
            P.op("dve", lambda g, lgt=lgt: g.tensor_tensor(out=lgt, in0=pb.t[:, 0:NE], in1=brt.t[:], op=ALU.add), reads=[pb.r, brt.r], writes=[rt.r])
            P.op("dve", lambda g, lgt=lgt, top8=top8: g.max(out=top8, in_=lgt), reads=[rt.r], writes=[rt.r])
            P.op("dve", lambda g, lgt=lgt, top8=top8, idu=idu: g.max_index(out=idu.t[:], in_max=top8, in_values=lgt), reads=[rt.r], writes=[idu.r])
            P.op("dve", lambda g, rt=rt: g.tensor_scalar(out=rt.t[:, 44:45], in0=rt.t[:, 32:33], scalar1=-1.0, scalar2=None, op0=ALU.mult), reads=[rt.r], writes=[rt.r])
            P.op("act", lambda g, rt=rt: g.activation(out=rt.t[:, 40:44], in_=rt.t[:, 32:36], func=AF.Exp, bias=rt.t[:, 44:45], scale=1.0, accum_out=rt.t[:, 45:46]), reads=[rt.r], writes=[rt.r])
            P.op("dve", lambda g, rt=rt: g.reciprocal(out=rt.t[:, 46:47], in_=rt.t[:, 45:46]), reads=[rt.r], writes=[rt.r])
            P.op("dve", lambda g, rt=rt, tb=tb: g.tensor_scalar(out=self.g4.t[:, tb, :], in0=rt.t[:, 40:44], scalar1=rt.t[:, 46:47], scalar2=None, op0=ALU.mult), reads=[rt.r], writes=[self.g4.r])
            P.op("dve", lambda g, rt=rt, sel=sel, lgt=lgt: g.tensor_scalar(out=sel.t[:], in0=lgt, scalar1=rt.t[:, 35:36], scalar2=None, op0=ALU.is_ge), reads=[rt.r], writes=[sel.r])
            pp = self.pb[3]
            P.op("pe", lambda g, sel=sel: g.matmul(pp.t[:, 0:NE], lhsT=self.ut.t[:], rhs=sel.t[:], start=True, stop=True), reads=[self.ut.r, sel.r], writes=[pp.r])
            P.op("dve", lambda g, rt=rt: g.tensor_tensor(out=rt.t[:, 96:128], in0=pp.t[:, 0:NE], in1=selsum.t[:], op=ALU.add), reads=[pp.r, selsum.r, rt.r], writes=[rt.r])
            P.op("dve", lambda g, rt=rt, idu=idu: g.tensor_copy(out=rt.t[:, 48:56], in_=idu.t[:]), reads=[idu.r], writes=[rt.r])
            for k in range(4):
                P.op("dve", lambda g, rt=rt, k=k: g.tensor_scalar(out=rt.t[:, 64:96], in0=self.iota32.t[:], scalar1=rt.t[:, 48 + k:49 + k], scalar2=None, op0=ALU.is_equal), reads=[rt.r, self.iota32.r], writes=[rt.r])
                P.op("dve", lambda g, rt=rt, k=k: g.scalar_tensor_tensor(out=rt.t[:, 64:96], in0=rt.t[:, 64:96], scalar=1.0, in1=rt.t[:, 96:128], op0=ALU.mult, op1=ALU.mult, accum_out=rt.t[:, 56 + k:57 + k]), reads=[rt.r], writes=[rt.r])
            P.op("dve", lambda g, rt=rt: g.tensor_scalar(out=rt.t[:, 128:132], in0=rt.t[:, 56:60], scalar1=float(CAP) - 0.5, scalar2=float(NE * CAP), op0=ALU.is_ge, op1=ALU.mult), reads=[rt.r], writes=[rt.r])
            P.op("dve", lambda g, rt=rt: g.scalar_tensor_tensor(out=rt.t[:, 132:136], in0=rt.t[:, 48:52], scalar=float(CAP), in1=rt.t[:, 56:60], op0=ALU.mult, op1=ALU.add), reads=[rt.r], writes=[rt.r])
            P.op("dve", lambda g, rt=rt: g.tensor_tensor(out=rt.t[:, 132:136], in0=rt.t[:, 132:136], in1=rt.t[:, 128:132], op=ALU.add), reads=[rt.r], writes=[rt.r])
            P.op("dve", lambda g, rt=rt, tb=tb: g.tensor_copy(out=self.idx4.t[:, tb, :], in_=rt.t[:, 132:136]), reads=[rt.r], writes=[self.idx4.r])
            pq = self.pb[1]
            P.op("pe", lambda g, sel=sel: g.matmul(pq.t[:, 0:NE], lhsT=self.ones32.t[:, 0:128], rhs=sel.t[:], start=True, stop=True), reads=[self.ones32.r, sel.r], writes=[pq.r])
            P.op("dve", lambda g: g.tensor_tensor(out=selsum.t[:], in0=selsum.t[:], in1=pq.t[:, 0:NE], op=ALU.add), reads=[selsum.r, pq.r], writes=[selsum.r])
            for k in range(4):
                it = ixr.next()
                P.op("dve", lambda g, rt=rt, it=it, k=k: g.tensor_copy(out=it.t[:, :], in_=rt.t[:, 132 + k:133 + k]), reads=[rt.r], writes=[it.r])
                P.dma("pool", lambda g, h2=h2, it=it: g.indirect_dma_start(out=self.XG.t[:, :], out_offset=bass.IndirectOffsetOnAxis(ap=it.t[:, :], axis=0), in_=h2.t[:, :], in_offset=None, bounds_check=self.bcreg(g, (128 if self.stub else NE * CAP) - 1), oob_is_err=False),
                      "xgsc", reads=[h2.r, it.r], writes=[self.XG.r])
        if "rt" in self.dbg:
            o3 = self.dram("rt", [128, 160], F32)
            self.outs.append(self.st(o3, o3.t.ap(), rt, rt.t[:], "dbg"))
            o4 = self.dram("idu", [128, 8], U32)
            self.outs.append(self.st(o4, o4.t.ap(), idu, idu.t[:], "dbg"))
        if "idx4" in self.dbg:
            o = self.dram("idx4", [128, NB * 4], I32)
            self.outs.append(self.st(o, o.t.ap(), self.idx4, self.idx4.t[:].rearrange("p n k -> p (n k)"), "dbg"))
            o2 = self.dram("g4", [128, NB * 4], F32)
            self.outs.append(self.st(o2, o2.t.ap(), self.g4, self.g4.t[:].rearrange("p n k -> p (n k)"), "dbg"))
        self.release()

    def phase_experts(self, l):
        nc, P = self.nc, self.P
        self.mark()
        ne_ = 1 if self.stub else NE
        W1r = Ring([self.sb([128, 8, 2 * D], BF16, "W1") for _ in range(2)])
        W2r = Ring([self.sb([128, 8, D], BF16, "W2") for _ in range(2)])
        b1r = Ring([self.sb([128, 16], F32, "b1") for _ in range(2)])
        b2r = Ring([self.sb([128, D], F32, "b2") for _ in range(2)])
        xgr = Ring([self.sb([128, 4, D], BF16, "xg") for _ in range(2)])
        xTr = Ring([self.sb([128, 8, 512], BF16, "xT") for _ in range(2)])
        uTr = Ring([self.sb([128, 8, 512], BF16, "uT") for _ in range(2)])
        gr = Ring([self.sb([128, 512], F32, "g") for _ in range(2)])
        sgr = Ring([self.sb([128, 512], F32, "sg") for _ in range(2)])
        ltr = Ring([self.sb([128, 512], F32, "lt") for _ in range(2)])
        ystr = Ring([self.sb([128, D], F32, "yst") for _ in range(2)])
        nexp = NE if not self.stub else 2
        tbk = 0
        for e in range(nexp):
            es = min(e, ne_ - 1)
            W1 = W1r.next(); W2 = W2r.next(); b1 = b1r.next(); b2 = b2r.next()
            w1b, w1ap = self.wsrc("w1", l, es)
            w2b, w2ap = self.wsrc("w2", l, es)
            w1src = w1ap.rearrange("(kc p) n -> p kc n", p=128)
            for hf in range(2):
                self.ld(W1, W1.t[:, hf * 4:(hf + 1) * 4, :], w1b, w1src[:, hf * 4:(hf + 1) * 4, :], "w1_%d" % (e % 2), q=self.wq)
            self.ld(W2, W2.t[:], w2b, w2ap.rearrange("(kc p) n -> p kc n", p=128), "w2_%d" % (e % 2), q=self.wq)
            self.ld(b1, b1.t[:], self.b1, self.b1.t.ap()[l, e], "b1_%d" % (e % 2))
            self.ld(b2, b2.t[:], self.b2, self.b2.t.ap()[l, e:e + 1, :].partition_broadcast(128), "b2_%d" % (e % 2))
            for ss in range(CAP // 512):
                r0 = e * CAP + ss * 512
                if self.stub:
                    r0 = 0
                xg = xgr.next(); xT = xTr.next(); uT = uTr.next()
                nj = 1 if self.stub else 4
                self.ld(xg, xg.t[:, 0:nj, :], self.XG, self.XG.t.ap()[r0:r0 + nj * 128, :].rearrange("(j p) d -> p j d", p=128), "ldxg%d" % (tbk % 2))
                for j in range(4):
                    jj = min(j, nj - 1)
                    self.transpose8(xg, xg.t[:, jj, :], 6 + (tbk % 2), xT, xT.t[:, :, j * 128:(j + 1) * 128])
                    tbk += 1
                for fc in range(8):
                    pg = self.pb[(fc % 2) * 2]; pl = self.pb[(fc % 2) * 2 + 1]
                    for kc in range(8):
                        P.op("pe", lambda g, pg=pg, kc=kc, fc=fc, W1=W1, xT=xT: g.matmul(pg.t[:, :], lhsT=W1.t[:, kc, fc * 128:(fc + 1) * 128], rhs=xT.t[:, kc, :], start=(kc == 0), stop=(kc == 7)), reads=[W1.r, xT.r], writes=[pg.r])
                    for kc in range(8):
                        P.op("pe", lambda g, pl=pl, kc=kc, fc=fc, W1=W1, xT=xT: g.matmul(pl.t[:, :], lhsT=W1.t[:, kc, D + fc * 128:D + (fc + 1) * 128], rhs=xT.t[:, kc, :], start=(kc == 0), stop=(kc == 7)), reads=[W1.r, xT.r], writes=[pl.r])
                    gt = gr.next(); sg = sgr.next(); lt = ltr.next()
                    P.op("dve", lambda g, pg=pg, gt=gt, b1=b1, fc=fc: g.tensor_scalar(out=gt.t[:], in0=pg.t[:, :], scalar1=b1.t[:, fc:fc + 1], scalar2=7.0, op0=ALU.add, op1=ALU.min), reads=[pg.r, b1.r], writes=[gt.r])
                    P.op("act", lambda g, gt=gt, sg=sg: g.activation(out=sg.t[:], in_=gt.t[:], func=AF.Sigmoid, scale=1.702), reads=[gt.r], writes=[sg.r])
                    P.op("dve", lambda g, pl=pl, lt=lt, b1=b1, fc=fc: g.tensor_scalar(out=lt.t[:], in0=pl.t[:, :], scalar1=b1.t[:, 8 + fc:9 + fc], scalar2=7.0, op0=ALU.add, op1=ALU.min), reads=[pl.r, b1.r], writes=[lt.r])
                    P.op("dve", lambda g, lt=lt: g.tensor_scalar(out=lt.t[:], in0=lt.t[:], scalar1=-7.0, scalar2=1.0, op0=ALU.max, op1=ALU.add), reads=[lt.r], writes=[lt.r])
                    P.op("dve", lambda g, gt=gt, sg=sg: g.tensor_tensor(out=gt.t[:], in0=gt.t[:], in1=sg.t[:], op=ALU.mult), reads=[gt.r, sg.r], writes=[gt.r])
                    P.op("dve", lambda g, gt=gt, lt=lt, uT=uT, fc=fc: g.tensor_tensor(out=uT.t[:, fc, :], in0=gt.t[:], in1=lt.t[:], op=ALU.mult), reads=[gt.r, lt.r], writes=[uT.r])
                for j in range(4):
                    yst = ystr.next()
                    for dc in range(2):
                        py = self.pb[4 + dc]
                        for fc in range(8):
                            P.op("pe", lambda g, py=py, fc=fc, dc=dc, j=j, uT=uT, W2=W2: g.matmul(py.t[:, :], lhsT=uT.t[:, fc, j * 128:(j + 1) * 128], rhs=W2.t[:, fc, dc * 512:(dc + 1) * 512], start=(fc == 0), stop=(fc == 7)), reads=[uT.r, W2.r], writes=[py.r])
                        P.op("dve", lambda g, py=py, dc=dc, yst=yst, b2=b2: g.tensor_tensor(out=yst.t[:, dc * 512:(dc + 1) * 512], in0=py.t[:, :], in1=b2.t[:, dc * 512:(dc + 1) * 512], op=ALU.add), reads=[py.r, b2.r], writes=[yst.r])
                    rr = (e * CAP + ss * 512 + j * 128) if not self.stub else 0
                    self.st(self.YG, self.YG.t.ap()[rr:rr + 128, :], yst, yst.t[:], "ygst")
        self.release()

    def phase_combine(self, l, xout):
        nc, P = self.nc, self.P
        self.mark()
        mod = self.mod
        lg = self.load_bc(self.ln_g, self.ln_g.t.ap()[l, 1:2, :], "lng")
        lb = self.load_bc(self.ln_b, self.ln_b.t.ap()[l, 1:2, :], "lnb")
        x1r = Ring([self.sb([128, D], F32, "x1") for _ in range(2)])
        ykr = Ring([self.sb([128, D], F32, "yk") for _ in range(6)])
        ixr = Ring([self.sb([128, 1], I32, "ix") for _ in range(8)])
        accr = Ring([self.sb([128, D], F32, "acc") for _ in range(2)])
        xor_ = Ring([self.sb([128, D], F32, "xo") for _ in range(2)])
        stt = Ring([self.sb([128, 32], F32, "st") for _ in range(3)])
        x1src = self.X1.t.ap().rearrange("(n p) d -> n p d", p=128)
        xdst = xout.t.ap().rearrange("(n p) d -> n p d", p=128)
        ng = 128 if self.stub else NE * CAP
        for tb in range(NB):
            x1 = x1r.next(); acc = accr.next(); xo = xor_.next()
            self.ld(x1, x1.t[:], self.X1, x1src[tb], "ldx1%d" % (tb % 2))
            for k in range(4):
                yk = ykr.next()
                P.op("pool", lambda g, yk=yk: g.memset(yk.t[:], 0.0), writes=[yk.r])
                it = ixr.next()
                P.op("dve", lambda g, it=it, tb=tb, k=k: g.tensor_copy(out=it.t[:, :], in_=self.idx4.t[:, tb, k:k + 1]), reads=[self.idx4.r], writes=[it.r])
                P.dma("pool", lambda g, yk=yk, it=it: g.indirect_dma_start(out=yk.t[:, :], out_offset=None, in_=self.YG.t[:, :], in_offset=bass.IndirectOffsetOnAxis(ap=it.t[:, :], axis=0), bounds_check=self.bcreg(g, ng - 1), oob_is_err=False),
                      "ygg%d" % ((tb * 4 + k) % 6), reads=[self.YG.r, it.r, yk.r], writes=[yk.r])
                if k == 0:
                    P.op("dve", lambda g, yk=yk, acc=acc, tb=tb: g.tensor_scalar(out=acc.t[:], in0=yk.t[:], scalar1=self.g4.t[:, tb, 0:1], scalar2=None, op0=ALU.mult), reads=[yk.r, self.g4.r], writes=[acc.r])
                else:
                    P.op("dve", lambda g, yk=yk, acc=acc, tb=tb, k=k: g.scalar_tensor_tensor(out=acc.t[:], in0=yk.t[:], scalar=self.g4.t[:, tb, k:k + 1], in1=acc.t[:], op0=ALU.mult, op1=ALU.add), reads=[yk.r, self.g4.r, acc.r], writes=[acc.r])
            P.op("pool", lambda g, acc=acc: g.tensor_tensor(out=acc.t[:], in0=acc.t[:], in1=mod.t[:, 5 * D:6 * D], op=ALU.mult), reads=[acc.r, mod.r], writes=[acc.r])
            P.op("dve", lambda g, acc=acc, x1=x1: g.scalar_tensor_tensor(out=acc.t[:], in0=x1.t[:], scalar=ALPHA, in1=acc.t[:], op0=ALU.mult, op1=ALU.add), reads=[x1.r, acc.r], writes=[acc.r])
            st = stt.next()
            self.ln_rows(acc, acc.t[:], st)
            P.op("act", lambda g, acc=acc, st=st: g.activation(out=acc.t[:], in_=acc.t[:], func=AF.Identity, bias=st.t[:, 9:10], scale=st.t[:, 8:9]), reads=[acc.r, st.r], writes=[acc.r])
            P.op("pool", lambda g, acc=acc: g.tensor_tensor(out=acc.t[:], in0=acc.t[:], in1=lg.t[:], op=ALU.mult), reads=[acc.r, lg.r], writes=[acc.r])
            P.op("dve", lambda g, acc=acc, xo=xo: g.tensor_tensor(out=xo.t[:], in0=acc.t[:], in1=lb.t[:], op=ALU.add), reads=[acc.r, lb.r], writes=[xo.r])
            o = self.st(xout, xdst[tb], xo, xo.t[:], "xost")
            if getattr(xout, 'final', False) or 'XL' in self.dbg:
                self.outs.append(o)
        self.release()


def prep_shared(inp):
    f = lambda a: np.ascontiguousarray(np.asarray(a, dtype=np.float32))
    w_in = f(inp["w_in"])
    seg = lambda a, b: w_in[:, :, a:b]
    fm = [seg(0, 256), seg(256, 512), seg(768, 1024), seg(1024, 1280), seg(1536, 1792), seg(1792, 2048),
          seg(2304, 3328), seg(3408, 3664), seg(3664, 3920), seg(3328, 3392)]
    vv = [seg(512, 768), seg(1280, 1536), seg(2048, 2304), seg(3920, 4176), seg(3392, 3408)]
    w_in_p = np.ascontiguousarray(np.concatenate(fm + vv, axis=2))
    w1 = f(inp["w1"])
    w1p = np.ascontiguousarray(np.concatenate([w1[..., 0::2], w1[..., 1::2]], axis=-1))
    b1 = f(inp["b1"])
    b1p = np.concatenate([b1[..., 0::2], b1[..., 1::2]], axis=-1)
    b1p = np.ascontiguousarray(b1p.reshape(DEPTH, NE, 16, 128).transpose(0, 1, 3, 2))
    ohb, ohc = _static_tables()
    return {
        "w_ada": f(inp["w_ada"]), "b_ada": f(inp["b_ada"]), "w_in": w_in_p, "w_out": f(inp["w_out"]),
        "diff_lam": f(inp["diff_lam"]).reshape(DEPTH, 128), "diff_g": f(inp["diff_g"]),
        "ln_g": f(inp["ln_g"]), "ln_b": f(inp["ln_b"]), "w_router": f(inp["w_router"]), "b_router": f(inp["b_router"]),
        "w1": w1p, "b1": b1p, "w2": f(inp["w2"]), "b2": f(inp["b2"]), "rel_bias": f(inp["rel_bias"]),
        "ohb": ohb, "ohc": ohc,
    }


def prep_core(inp, b, shared=None, shard=False, nb=1):
    x = np.ascontiguousarray(np.asarray(inp["x"][b * nb:(b + 1) * nb], dtype=np.float32))
    c = np.asarray(inp["c"][b * nb:(b + 1) * nb], dtype=np.float32)
    cbc = np.ascontiguousarray(np.broadcast_to(c.reshape(nb, 8, 128).transpose(0, 2, 1)[:, :, :, None], (nb, 128, 8, 128)))
    m = {"x": x, "cbc": cbc}
    if shard:
        for k in ("w_ada", "w_in", "w_out"):
            m[k] = np.ascontiguousarray(shared[k][:, 128 * b:128 * (b + 1), :])
        for k in ("w1", "w2"):
            m[k] = np.ascontiguousarray(shared[k][:, 4 * b:4 * (b + 1)])
    return m


_NC_CACHE = {}


NCORES = 8
NBAT = 8 // NCORES


def kernel(**inputs):
    if "nc" not in _NC_CACHE:
        _NC_CACHE["nc"] = Builder(nb=NBAT).build()
    nc = _NC_CACHE["nc"]
    shared = prep_shared(inputs)
    in_maps = []
    for c in range(NCORES):
        m = dict(shared)
        m.update(prep_core(inputs, c, nb=NBAT))
        in_maps.append(m)
    res = run_bass_kernel_spmd(nc, in_maps, core_ids=list(range(NCORES)))
    return np.concatenate([np.asarray(r["out"], dtype=np.float32) for r in res.results], axis=0)
```
